# Optimizing a Trainium2 kernel written in Bass

```python
import jax, jax.numpy as jnp
from jax import lax
import numpy as np

D_MODEL = 1024
BATCH = 16
SEQ = 2048
DEPTH = 1

PLE_DIM = 256
D_MIX = D_MODEL
MLSTM_HEADS = 4
MLSTM_HEAD_DIM = 128
MLSTM_WIDTH = MLSTM_HEADS * MLSTM_HEAD_DIM
HGRN_HEADS = 4
HGRN_HEAD_DIM = 128
HGRN_WIDTH = HGRN_HEADS * HGRN_HEAD_DIM
CONV_WIDTH = 4
MLSTM_CHUNK = 128
HGRN_CHUNK = 32
MLSTM_COLS = 4 * MLSTM_WIDTH + 2 * MLSTM_HEADS
HGRN_COLS = 4 * HGRN_WIDTH
IN_COLS = MLSTM_COLS + HGRN_COLS
N_GROUPS = 4
EXPERTS_PER_GROUP = 8
N_EXPERTS = N_GROUPS * EXPERTS_PER_GROUP
TOP_K = 2
D_EXPERT = 512
MOE_BLOCK = 128
EPS = 1e-6

kernel_name = "hybrid_mlstm_hgrn2_hmoe_block"


def rms_norm(u, g):
    u32 = u.astype(jnp.float32)
    y = u32 * lax.rsqrt(jnp.mean(u32 * u32, axis=-1, keepdims=True) + EPS) * g.astype(jnp.float32)
    return y.astype(u.dtype)


def to_chunks(u, n_heads, chunk):
    b, t, _ = u.shape
    return u.reshape(b, t // chunk, chunk, n_heads, -1).transpose(1, 0, 3, 2, 4)


def gates_to_chunks(u, chunk):
    b, t, h = u.shape
    return u.reshape(b, t // chunk, chunk, h).transpose(1, 0, 3, 2)


def from_chunks(u):
    nc, b, h, l, d = u.shape
    return u.transpose(1, 0, 3, 2, 4).reshape(b, nc * l, h, d)


def causal_depthwise_conv(u, w):
    k, c = w.shape
    return lax.conv_general_dilated(u, w[:, None, :].astype(u.dtype), window_strides=(1,),
                                    padding=[(k - 1, 0)], dimension_numbers=("NWC", "WIO", "NWC"),
                                    feature_group_count=c)


def mlstm_mixer(qk_pre, v_in, o_pre, gate_pre, conv_w, g_norm):
    b, t, _ = v_in.shape
    qk = jax.nn.silu(causal_depthwise_conv(qk_pre, conv_w)).astype(jnp.float32)
    q = qk[..., :MLSTM_WIDTH]
    k = qk[..., MLSTM_WIDTH:] * (MLSTM_HEAD_DIM ** -0.5)
    v = v_in.astype(jnp.float32)
    ig = gate_pre[..., :MLSTM_HEADS]
    lf = jax.nn.log_sigmoid(gate_pre[..., MLSTM_HEADS:])
    qc, kc, vc = (to_chunks(u, MLSTM_HEADS, MLSTM_CHUNK) for u in (q, k, v))
    igc, lfc = gates_to_chunks(ig, MLSTM_CHUNK), gates_to_chunks(lf, MLSTM_CHUNK)
    mask = jnp.tril(jnp.ones((MLSTM_CHUNK, MLSTM_CHUNK), bool))

    def step(carry, xs):
        c_st, n_st, m_st = carry
        qq, kk, vv, ii, ff = xs
        bcum = jnp.cumsum(ff, axis=-1)
        log_d = jnp.where(mask, bcum[..., :, None] - bcum[..., None, :] + ii[..., None, :], -jnp.inf)
        inter = bcum + m_st[..., None]
        m_t = jnp.maximum(inter, jnp.max(log_d, axis=-1))
        s = jnp.einsum("bhtd,bhsd->bhts", qq, kk) * jnp.exp(log_d - m_t[..., None])
        w_inter = jnp.exp(inter - m_t)
        num = jnp.einsum("bhts,bhse->bhte", s, vv) + w_inter[..., None] * jnp.einsum("bhtd,bhde->bhte", qq, c_st)
        den = jnp.sum(s, axis=-1) + w_inter * jnp.einsum("bhtd,bhd->bht", qq, n_st)
        h = num / jnp.maximum(jnp.abs(den), jnp.exp(-m_t))[..., None]
        b_last = bcum[..., -1]
        w_log = b_last[..., None] - bcum + ii
        m_new = jnp.maximum(b_last + m_st, jnp.max(w_log, axis=-1))
        w = jnp.exp(w_log - m_new[..., None])
        decay = jnp.exp(b_last + m_st - m_new)
        c_new = decay[..., None, None] * c_st + jnp.einsum("bhs,bhsd,bhse->bhde", w, kk, vv)
        n_new = decay[..., None] * n_st + jnp.einsum("bhs,bhsd->bhd", w, kk)
        return (c_new, n_new, m_new), h

    init = (jnp.zeros((b, MLSTM_HEADS, MLSTM_HEAD_DIM, MLSTM_HEAD_DIM), jnp.float32),
            jnp.zeros((b, MLSTM_HEADS, MLSTM_HEAD_DIM), jnp.float32),
            jnp.zeros((b, MLSTM_HEADS), jnp.float32))
    _, hc = lax.scan(step, init, (qc, kc, vc, igc, lfc))
    h = from_chunks(hc)
    h = h * jax.nn.sigmoid(o_pre.astype(jnp.float32)).reshape(h.shape)
    h = h * lax.rsqrt(jnp.mean(h * h, axis=-1, keepdims=True) + EPS)
    return (h.reshape(b, t, MLSTM_WIDTH) * g_norm.astype(jnp.float32)).astype(v_in.dtype)


def hgrn2_mixer(q_pre, f_pre, i_in, g_pre, lb, g_norm):
    b, t, _ = q_pre.shape
    q = jax.nn.silu(q_pre.astype(jnp.float32))
    f32p = f_pre.astype(jnp.float32)
    f = lb + (1.0 - lb) * jax.nn.sigmoid(f32p)
    k = (1.0 - lb) * jax.nn.sigmoid(-f32p)
    lf = jnp.log(f)
    v = i_in.astype(jnp.float32)
    qc, kc, vc, lfc = (to_chunks(u, HGRN_HEADS, HGRN_CHUNK) for u in (q, k, v, lf))
    mask = jnp.tril(jnp.ones((HGRN_CHUNK, HGRN_CHUNK), bool))[:, :, None]

    def step(s_st, xs):
        qq, kk, vv, ll = xs
        bcum = jnp.cumsum(ll, axis=2)
        diff = bcum[:, :, :, None, :] - bcum[:, :, None, :, :]
        decay = jnp.exp(jnp.where(mask, diff, -jnp.inf))
        a = jnp.einsum("bhtd,bhsd,bhtsd->bhts", qq, kk, decay)
        o = jnp.einsum("bhts,bhse->bhte", a, vv) + jnp.einsum("bhtd,bhde->bhte", qq * jnp.exp(bcum), s_st)
        b_last = bcum[:, :, -1:, :]
        s_new = jnp.exp(b_last[:, :, 0, :])[..., None] * s_st + \
            jnp.einsum("bhsd,bhse->bhde", kk * jnp.exp(b_last - bcum), vv)
        return s_new, o

    s0 = jnp.zeros((b, HGRN_HEADS, HGRN_HEAD_DIM, HGRN_HEAD_DIM), jnp.float32)
    _, oc = lax.scan(step, s0, (qc, kc, vc, lfc))
    o = from_chunks(oc)
    o = o * lax.rsqrt(jnp.mean(o * o, axis=-1, keepdims=True) + EPS) * g_norm.astype(jnp.float32)
    o = o.reshape(b, t, HGRN_WIDTH) * jax.nn.silu(g_pre.astype(jnp.float32))
    return o.astype(q_pre.dtype)


def hier_moe(h, w_rg, b_rg, w_re, b_re, w_gate, w_up, w_down):
    b, t, d = h.shape
    n_tok = b * t
    hf = h.reshape(n_tok, d)
    h32 = hf.astype(jnp.float32)
    g_logits = h32 @ w_rg.astype(jnp.float32) + b_rg.astype(jnp.float32)
    g_prob = jax.nn.softmax(g_logits, axis=-1)
    g_val, g_sel = lax.top_k(g_prob, 1)
    e_logits = (h32 @ w_re.astype(jnp.float32) + b_re.astype(jnp.float32)).reshape(n_tok, N_GROUPS, EXPERTS_PER_GROUP)
    e_in_group = jnp.take_along_axis(e_logits, g_sel[:, :, None], axis=1)[:, 0]
    top_v, top_i = lax.top_k(e_in_group, TOP_K)
    comb = jax.nn.softmax(top_v, axis=-1) * g_val
    expert_id = g_sel * EXPERTS_PER_GROUP + top_i

    n_asg = n_tok * TOP_K
    flat_e = expert_id.reshape(n_asg).astype(jnp.int32)
    flat_w = comb.reshape(n_asg)
    flat_tok = jnp.arange(n_asg, dtype=jnp.int32) // TOP_K
    order = jnp.argsort(flat_e)
    se, st, sw = flat_e[order], flat_tok[order], flat_w[order]
    counts = jnp.bincount(flat_e, length=N_EXPERTS).astype(jnp.int32)
    padded = (counts + MOE_BLOCK - 1) // MOE_BLOCK * MOE_BLOCK
    start = jnp.cumsum(counts) - counts
    pend = jnp.cumsum(padded)
    pstart = pend - padded
    dest = pstart[se] + jnp.arange(n_asg, dtype=jnp.int32) - start[se]
    n_blocks = -(-n_asg // MOE_BLOCK) + N_EXPERTS
    n_rows = n_blocks * MOE_BLOCK
    row_tok = jnp.full((n_rows,), n_tok, jnp.int32).at[dest].set(st)
    row_w = jnp.zeros((n_rows,), jnp.float32).at[dest].set(sw)
    block_e = jnp.minimum(jnp.searchsorted(pend, jnp.arange(n_blocks, dtype=jnp.int32) * MOE_BLOCK, side="right"),
                          N_EXPERTS - 1)
    x_pad = jnp.concatenate([hf, jnp.zeros((1, d), hf.dtype)], axis=0)
    xb = x_pad[row_tok].reshape(n_blocks, MOE_BLOCK, d)

    def expert_block(args):
        xblk, e = args
        return (jax.nn.silu(xblk @ w_gate[e]) * (xblk @ w_up[e])) @ w_down[e]

    yb = lax.map(expert_block, (xb, block_e)).reshape(n_rows, d)
    y = jax.ops.segment_sum(yb * row_w[:, None].astype(yb.dtype), row_tok, num_segments=n_tok + 1)[:n_tok]
    return y.reshape(b, t, d).astype(h.dtype)


def setup_inputs(seed: int = 0) -> dict:
    key = jax.random.key(seed)
    ks = jax.random.split(key, 24)
    f32 = jnp.float32

    def nrm(k, shape, scale):
        return jax.random.normal(k, shape, f32) * scale

    b_i = nrm(ks[4], (DEPTH, MLSTM_HEADS), 0.1)
    b_f = jnp.linspace(3.0, 6.0, MLSTM_HEADS, dtype=f32)[None, :] + nrm(ks[5], (DEPTH, MLSTM_HEADS), 0.1)
    return {
        "x": nrm(ks[0], (BATCH, SEQ, D_MODEL), 1.0),
        "p": nrm(ks[1], (DEPTH, BATCH, SEQ, PLE_DIM), 1.0),
        "g_mix": 1.0 + nrm(ks[2], (DEPTH, D_MODEL), 0.02),
        "w_in": nrm(ks[3], (DEPTH, D_MODEL, IN_COLS), D_MODEL ** -0.5),
        "b_mgate": jnp.concatenate([b_i, b_f], axis=-1),
        "conv_qk": nrm(ks[6], (DEPTH, CONV_WIDTH, 2 * MLSTM_WIDTH), CONV_WIDTH ** -0.5),
        "g_mlstm": 1.0 + nrm(ks[7], (DEPTH, MLSTM_WIDTH), 0.02),
        "hg_lb": nrm(ks[8], (DEPTH + 1, HGRN_WIDTH), 0.5),
        "g_hgrn": 1.0 + nrm(ks[9], (DEPTH, HGRN_HEAD_DIM), 0.02),
        "w_out": nrm(ks[10], (DEPTH, D_MIX, D_MODEL), D_MIX ** -0.5),
        "g_ffn": 1.0 + nrm(ks[11], (DEPTH, D_MODEL), 0.02),
        "w_rg": nrm(ks[12], (DEPTH, D_MODEL, N_GROUPS), D_MODEL ** -0.5),
        "b_rg": nrm(ks[13], (DEPTH, N_GROUPS), 0.01),
        "w_re": nrm(ks[14], (DEPTH, D_MODEL, N_EXPERTS), D_MODEL ** -0.5),
        "b_re": nrm(ks[15], (DEPTH, N_EXPERTS), 0.01),
        "w_e_gate": nrm(ks[16], (DEPTH, N_EXPERTS, D_MODEL, D_EXPERT), D_MODEL ** -0.5),
        "w_e_up": nrm(ks[17], (DEPTH, N_EXPERTS, D_MODEL, D_EXPERT), D_MODEL ** -0.5),
        "w_e_down": nrm(ks[18], (DEPTH, N_EXPERTS, D_EXPERT, D_MODEL), D_EXPERT ** -0.5),
        "g_pl": 1.0 + nrm(ks[19], (DEPTH, D_MODEL), 0.02),
        "w_pl_gate": nrm(ks[20], (DEPTH, D_MODEL, D_MODEL), D_MODEL ** -0.5),
        "w_pl_proj": nrm(ks[21], (DEPTH, PLE_DIM, D_MODEL), PLE_DIM ** -0.5),
        "g_final": 1.0 + nrm(ks[22], (D_MODEL,), 0.02),
    }


def reference(x, p, g_mix, w_in, b_mgate, conv_qk, g_mlstm, hg_lb, g_hgrn, w_out, g_ffn,
              w_rg, b_rg, w_re, b_re, w_e_gate, w_e_up, w_e_down, g_pl, w_pl_gate, w_pl_proj, g_final):
    lower_bounds = jnp.cumsum(jax.nn.softmax(hg_lb.astype(jnp.float32), axis=0), axis=0)
    w = MLSTM_WIDTH
    for i in range(DEPTH):
        h = rms_norm(x, g_mix[i])
        z = h @ w_in[i]
        gate_pre = z[..., 4 * w:MLSTM_COLS].astype(jnp.float32) + b_mgate[i].astype(jnp.float32)
        y_m = mlstm_mixer(z[..., :2 * w], z[..., 2 * w:3 * w], z[..., 3 * w:4 * w], gate_pre,
                          conv_qk[i], g_mlstm[i])
        o = MLSTM_COLS
        y_h = hgrn2_mixer(z[..., o:o + HGRN_WIDTH], z[..., o + HGRN_WIDTH:o + 2 * HGRN_WIDTH],
                          z[..., o + 2 * HGRN_WIDTH:o + 3 * HGRN_WIDTH], z[..., o + 3 * HGRN_WIDTH:o + 4 * HGRN_WIDTH],
                          lower_bounds[i], g_hgrn[i])
        x = x + jnp.concatenate([y_m, y_h], axis=-1) @ w_out[i]
        x = x + hier_moe(rms_norm(x, g_ffn[i]), w_rg[i], b_rg[i], w_re[i], b_re[i],
                         w_e_gate[i], w_e_up[i], w_e_down[i])
        x = x + jax.nn.sigmoid(rms_norm(x, g_pl[i]) @ w_pl_gate[i]) * (p[i] @ w_pl_proj[i])
    return rms_norm(x, g_final)
```

```python
import numpy as np
from contextlib import ExitStack
import concourse.bass as bass
import concourse.mybir as mybir
from concourse.bass_utils import run_bass_kernel_spmd

F32 = mybir.dt.float32
BF16 = mybir.dt.bfloat16
I32 = mybir.dt.int32
AF = mybir.ActivationFunctionType
ALU = mybir.AluOpType
AX = mybir.AxisListType

ENGS = ("pe", "act", "dve", "pool", "sp")
EPS = 1e-6
D = 1024
NE = 32
DE = 512


class Sched:
    def __init__(self, nc, es, n_dma_sems=80, strict_same=True):
        self.nc = nc
        self.ops = {e: [] for e in ENGS}
        self.cnt = {}
        self.sem = {}
        self.seen = {}
        self.res = {}
        self.strict_same = strict_same
        for e in ENGS:
            self.sem[e] = es.enter_context(nc.semaphore("s_" + e))
            self.cnt[e] = 0
        self.free_sems = [es.enter_context(nc.semaphore("d%d" % i)) for i in range(n_dma_sems)]
        self.final_waits = []
        self.cap = None
        self.deferred = []
        self.pumping = False

    def _r(self, key):
        r = self.res.get(key)
        if r is None:
            r = self.res[key] = {"w": None, "r": {}}
        return r

    def _deps(self, reads, writes):
        deps = []
        for k in reads:
            r = self._r(k)
            if r["w"] is not None:
                deps.append(r["w"])
            if isinstance(k, tuple) and k[0] in ("pb", "pt"):
                deps.extend(r["r"].items())
        for k in writes:
            r = self._r(k)
            if r["w"] is not None:
                deps.append(r["w"])
            deps.extend(r["r"].items())
        return deps

    def _waits(self, eng, deps):
        best = {}
        for (x, v) in deps:
            if x == eng and (not self.strict_same or eng == "pe"):
                continue
            if v > best.get(x, 0):
                best[x] = v
        out = []
        for x, v in best.items():
            if v > self.seen.get((eng, x), 0):
                self.seen[(eng, x)] = v
                out.append((self.sem[x], v))
        return out

    def _commit(self, stamp, reads, writes):
        x, v = stamp
        for k in reads:
            r = self._r(k)
            if r["r"].get(x, 0) < v:
                r["r"][x] = v
        for k in writes:
            r = self._r(k)
            r["w"] = (x, v)
            r["r"] = {}

    def op(self, eng, fn, reads=(), writes=(), sig=True):
        if self.cap is not None:
            self.cap.append(lambda: self.op(eng, fn, reads, writes, sig))
            return
        self._op(eng, fn, reads, writes, sig)
        if eng == "dve" and self.deferred and not self.pumping:
            self.pump(1)

    def pump(self, n):
        self.pumping = True
        while n > 0 and self.deferred:
            self.deferred.pop(0)()
            n -= 1
        self.pumping = False

    def capture(self):
        sch = self

        class _C:
            def __enter__(s_):
                s_.prev = sch.cap
                sch.cap = []
                s_.lst = sch.cap
                return s_.lst

            def __exit__(s_, *a):
                sch.cap = s_.prev
                return False
        return _C()

    def call(self, fn):
        if self.cap is not None:
            self.cap.append(fn)
        else:
            fn()

    def _op(self, eng, fn, reads=(), writes=(), sig=True):
        waits = self._waits(eng, self._deps(reads, writes))
        if sig:
            self.cnt[eng] += 1
            stamp = (eng, self.cnt[eng])
            inc = (self.sem[eng], 1)
        else:
            stamp = (eng, self.cnt[eng] + 1)
            inc = None
        self.ops[eng].append((waits, fn, inc))
        self._commit(stamp, reads, writes)

    def dma(self, q, fn, reads=(), writes=(), semkey=None, final=False):
        if self.cap is not None:
            self.cap.append(lambda: self.dma(q, fn, reads, writes, semkey, final))
            return
        if semkey is None:
            semkey = ("dma", (tuple(writes) + tuple(reads))[0])
        if semkey not in self.sem:
            self.sem[semkey] = self.free_sems.pop()
            self.cnt[semkey] = 0
        waits = self._waits(q, self._deps(reads, writes))
        self.cnt[semkey] += 16
        stamp = (semkey, self.cnt[semkey])
        self.ops[q].append((waits, fn, (self.sem[semkey], 16)))
        self._commit(stamp, reads, writes)
        if final:
            self.final_waits.append(stamp)

    def barrier(self):
        self.pump(10 ** 9)
        tot = [(x, v) for x, v in self.cnt.items() if v > 0]
        for e in ENGS:
            waits = self._waits(e, tot)
            if waits:
                self.ops[e].append((waits, None, None))
        self.res = {}

    def flush(self):
        nc = self.nc
        fin = [(self.sem[x], v) for x, v in self.cnt.items() if v > 0 and x != "sp"]
        ops = self.ops

        def run(engine, lst, tail=()):
            for (waits, fn, inc) in lst:
                for (s, v) in waits:
                    engine.wait_ge(s, v)
                if fn is None:
                    continue
                ins = fn(engine)
                if inc is not None:
                    ins.then_inc(inc[0], inc[1])
            for (s, v) in tail:
                engine.wait_ge(s, v)

        with nc.Block() as block:
            @block.sync
            def _(e):
                run(e, ops["sp"], fin)

            @block.scalar
            def _(e):
                run(e, ops["act"])

            @block.vector
            def _(e):
                run(e, ops["dve"])

            @block.gpsimd
            def _(e):
                run(e, ops["pool"])

            @block.tensor
            def _(e):
                run(e, ops["pe"])


class Ring:
    sched = None

    def __init__(self, name, aps):
        self.name, self.t, self.i = name, aps, -1

    def next(self):
        self.i = (self.i + 1) % len(self.t)
        key = (self.name, self.i)
        S_ = Ring.sched
        if S_ is not None and S_.cap is None and self.name in ("pb", "pt"):
            r = S_.res.get(key)
            assert r is None or r["w"] is None or len(r["r"]) > 0, ("PSUM ring slot reused before its content was read", key)
        return self.t[self.i], key


class Arena:
    def __init__(self, t, n):
        self.t, self.n, self.off = t, n, 0

    def reset(self):
        self.off = 0

    def get(self, shape):
        n = int(np.prod(shape[1:]))
        n_al = (n + 15) // 16 * 16
        assert self.off + n_al <= self.n, ("arena overflow", self.off, n_al, self.n)
        ap = self.t[:, self.off:self.off + n]
        self.off += n_al
        if len(shape) == 3:
            ap = ap.rearrange("p (a b) -> p a b", a=shape[1])
        elif len(shape) == 4:
            ap = ap.rearrange("p (a b c) -> p a b c", a=shape[1], b=shape[2])
        return ap

    def ring(self, name, shape, n):
        return Ring(name, [self.get(shape) for _ in range(n)])


def build_nc(NT, TPS, CAP, dbg=False):
    NTOK = NT * 128
    NB = CAP // 128
    ZROW = NE * CAP
    nc = bass.Bass("TRN2", target_bir_lowering=False)

    def din(name, shape, dt=F32):
        return nc.dram_tensor(name, list(shape), dt, kind="ExternalInput").ap()

    x_d = din("x", [NTOK, D]); p_d = din("p", [NTOK, 256])
    w_in = din("w_in", [D, 4104]); w_out = din("w_out", [D, D])
    wrt_d = din("wrt", [D, 36]); brt_d = din("brt", [1, 36])
    cw_d = din("cw", [128, 8, 4]); hl_d = din("hl", [128, 4, 2])
    g_mix = din("g_mix", [1, D]); g_ffn = din("g_ffn", [1, D]); g_pl = din("g_pl", [1, D]); g_fin = din("g_final", [1, D])
    g_ml = din("g_mlstm", [1, 512]); g_hg = din("g_hgrn", [1, 128]); bg_d = din("b_mgate", [1, 8])
    weg = din("w_e_gate", [NE, D, DE]); weu = din("w_e_up", [NE, D, DE]); wed = din("w_e_down", [NE, DE, D])
    wplg = din("w_pl_gate", [D, D]); wplp = din("w_pl_proj", [256, D])
    c_ident = din("c_ident", [128, 128]); c_tri = din("c_tri", [128, 128]); c_blk = din("c_blk", [128, 128])
    c_stri = din("c_stri", [128, 128]); c_rmask = din("c_rmask", [128, 512])
    c_mt = din("c_mt", [128, 4, 128]); c_ms = din("c_ms", [128, 4]); c_ec = din("c_ec", [128, 32])
    out_d = nc.dram_tensor("out", [NTOK, D], F32, kind="ExternalOutput").ap()
    x1s = nc.dram_tensor("x1s", [NTOK, D], F32, kind="Internal").ap()
    xs_d = nc.dram_tensor("xs", [ZROW + 1, D], BF16, kind="Internal").ap()
    yb_d = nc.dram_tensor("yb", [ZROW + 1, D], F32, kind="Internal").ap()
    dbg_d = {}
    if dbg:
        for nm, shp in (("d_y", [NTOK, D]), ("d_x1", [NTOK, D]), ("d_lg", [NTOK, 36]), ("d_slot", [NTOK, 4])):
            dbg_d[nm] = nc.dram_tensor(nm, shp, F32, kind="ExternalOutput").ap()

    with ExitStack() as es:
        S = Sched(nc, es)
        Ring.sched = S
        sbt = lambda name, shape, dt: es.enter_context(nc.sbuf_tensor("sb_" + name, shape, dt))
        Wi = sbt("Wi", [128, 8 * 4104], BF16)
        Wi3 = Wi[:, :].rearrange("p (k n) -> p k n", k=8)
        Wo = sbt("Wo", [128, 8 * D], BF16)
        Wo3 = Wo[:, :].rearrange("p (k n) -> p k n", k=8)
        Wr = sbt("Wr", [128, 8, 36], BF16)
        gA = sbt("gA", [128, D], F32)
        gB = sbt("gB", [128, D], F32)
        gm_bc = sbt("gm_bc", [128, 512], F32)
        gh_bc = sbt("gh_bc", [128, 128], F32)
        bg_bc = sbt("bg_bc", [128, 8], F32)
        brt_bc = sbt("brt_bc", [128, 36], F32)
        cw = sbt("cw", [128, 8, 4], F32)
        hl = sbt("hl", [128, 4, 2], F32)
        lbp = sbt("lbp", [128, 4, 2], F32)
        ident = sbt("ident", [128, 128], BF16)
        tri = sbt("tri", [128, 128], F32)
        onesf = sbt("onesf", [128, 128], F32)
        blk = sbt("blk", [128, 128], F32)
        stri = sbt("stri", [128, 128], BF16)
        onesb = sbt("onesb", [128, 128], BF16)
        rmask = sbt("rmask", [128, 512], F32)
        mt = sbt("mt", [128, 4, 128], BF16)
        ms = sbt("ms", [128, 4], F32)
        ec = sbt("ec", [128, 32], F32)
        d1i = sbt("d1i", [128, NT], I32); d2i = sbt("d2i", [128, NT], I32)
        cw1 = sbt("cw1", [128, NT], F32); cw2 = sbt("cw2", [128, NT], F32)
        base = sbt("base", [128, 32], F32)
        zrow = sbt("zrow", [128, 8], F32)
        NF, NBF = (12300 if dbg else 11300), 25900
        AFt = sbt("arenaF", [128, NF], F32); ABt = sbt("arenaB", [128, NBF], BF16)
        AFa, ABa = Arena(AFt, NF), Arena(ABt, NBF)
        pb = Ring("pb", [es.enter_context(nc.psum_tensor("pb%d" % i, [128, 512], F32)) for i in range(5)])
        rbank = es.enter_context(nc.psum_tensor("rbank", [128, 512], F32))
        pt = Ring("pt", [es.enter_context(nc.psum_tensor("pt%d" % i, [128, 1024], BF16)) for i in range(2)])

        def V(eng, name, reads, writes, **kw):
            S.op(eng, lambda e: getattr(e, name)(**kw), reads, writes)

        def ACTV(reads, writes, **kw):
            S.op("act", lambda e: e.activation(**kw), reads, writes)

        def MM(out, pairs, reads, writes):
            n = len(pairs)
            for i, (l, r) in enumerate(pairs):
                S.op("pe", lambda e, l=l, r=r, i=i: e.matmul(out, lhsT=l, rhs=r, start=(i == 0), stop=(i == n - 1)),
                     reads, writes, sig=(i == n - 1))

        def TR(out, in_, reads, writes, sig):
            S.op("pe", lambda e: e.transpose(out=out, in_=in_, identity=ident[:]), list(reads) + ["ident"], writes, sig=sig)

        def LD(q, out, in_, writes, reads=()):
            S.dma(q, lambda e: e.dma_start(out=out, in_=in_), reads, writes)

        def transpose8(src, ksrc, dst, kdst, evac_eng):
            P, kp = pt.next()
            for k in range(8):
                TR(P[:, k * 128:(k + 1) * 128], src[:, k * 128:(k + 1) * 128], [ksrc], [kp], sig=(k == 7))
            if evac_eng == "act":
                S.op("act", lambda e: e.copy(out=dst, in_=P[:, :].rearrange("p (k t) -> p k t", k=8)), [kp], [kdst])
            else:
                V("dve", "tensor_copy", [kp], [kdst], out=dst, in_=P[:, :].rearrange("p (k t) -> p k t", k=8))

        def rstd_from_ss(ss, kss, n, width):
            ACTV([kss], [kss], out=ss, in_=ss, func=AF.Ln, scale=1.0 / n, bias=EPS)
            ACTV([kss], [kss], out=ss, in_=ss, func=AF.Exp, scale=-0.5)

        for k in range(8):
            LD("pool", Wi3[:, k, :], w_in[k * 128:(k + 1) * 128, :], [("Wi", k)])
        LD("sp", gA[:], g_mix.partition_broadcast(128), ["gA"])
        LD("sp", gB[:], g_ffn.partition_broadcast(128), ["gB"])
        LD("sp", gm_bc[:], g_ml.partition_broadcast(128), ["gm_bc"])
        LD("sp", gh_bc[:], g_hg.partition_broadcast(128), ["gh_bc"])
        LD("sp", bg_bc[:], bg_d.partition_broadcast(128), ["bg_bc"])
        LD("sp", brt_bc[:], brt_d.partition_broadcast(128), ["brt_bc"])
        LD("sp", cw[:], cw_d, ["cw"]); LD("sp", hl[:], hl_d, ["hl"])
        LD("sp", tri[:], c_tri, ["tri"]); LD("sp", blk[:], c_blk, ["blk"]); LD("sp", rmask[:], c_rmask, ["rmask"])
        LD("sp", ec[:], c_ec, ["ec"])
        LD("pool", ident[:], c_ident, ["ident"]); LD("pool", stri[:], c_stri, ["stri"])
        LD("pool", mt[:], c_mt, ["mt"]); LD("sp", ms[:], c_ms, ["ms"])
        LD("pool", Wo3[:, :, :], w_out.rearrange("(k p) n -> p k n", p=128), ["Wo"])
        LD("pool", Wr[:], wrt_d.rearrange("(k p) n -> p k n", p=128), ["Wr"])
        V("pool", "memset", [], ["onesf"], ap=onesf[:], constant=1.0)
        V("pool", "memset", [], ["onesb"], ap=onesb[:], constant=1.0)
        V("pool", "memset", [], ["base"], ap=base[:], constant=0.0)
        V("pool", "memset", [], ["zrow"], ap=zrow[:], constant=0.0)
        S.dma("sp", lambda e: e.dma_start(out=yb_d[ZROW:ZROW + 1, :].rearrange("o (p f) -> (o p) f", p=128), in_=zrow[:]), ["zrow"], ["yb_z"])
        V("dve", "tensor_tensor", ["hl"], ["lbp"], out=lbp[:, :, 0], in0=hl[:, :, 0], in1=hl[:, :, 1], op=ALU.subtract)
        ACTV(["lbp"], ["lbp"], out=lbp[:, :, 0], in_=lbp[:, :, 0], func=AF.Sigmoid)
        V("dve", "tensor_scalar", ["lbp"], ["lbp"], out=lbp[:, :, 1], in0=lbp[:, :, 0], scalar1=-1.0, scalar2=1.0,
          op0=ALU.mult, op1=ALU.add)

        xt_r = AFa.ring("xt", [128, D], 2)
        zqk = AFa.get([128, 8, 131]); tailb = AFa.get([128, 8, 3]); acc = AFa.get([128, 8, 128])
        qs = AFa.get([128, 512]); sgf = AFa.get([128, 512]); lfkk = AFa.get([128, 1024]); lfb = lfkk[:, 0:512]; kk = lfkk[:, 512:1024]
        bb = AFa.get([128, 512]); sig_o_r = [AFa.get([128, 512]) for _ in range(2)]; gs_r = [AFa.get([128, 512]) for _ in range(2)]
        hmog = AFa.get([128, 1024]); og = hmog[:, 512:1024]; ctmp = lfkk.rearrange("p (c t) -> p c t", c=8)
        hm = hmog[:, 0:512].rearrange("p (h e) -> p h e", h=4); Cst = AFa.get([128, 4, 129]); Sf = AFa.get([128, 4, 128])
        sm = AFa.get([128, 128])
        lg = AFa.get([128, 36]); mskt = AFa.get([128, 32]); sel1 = AFa.get([128, 32]); sel2 = AFa.get([128, 32])
        pos = AFa.get([128, 32]); tmp32 = AFa.get([128, 32]); top8 = AFa.get([128, 8])
        hb = ABa.get([128, D]); hT_r = ABa.ring("hT", [128, 8, 128], 2)
        qkT = ABa.get([128, 8, 128]); vext_r = [ABa.get([128, 4, 130]) for _ in range(2)]; hvb_r = [ABa.get([128, 512]) for _ in range(2)]
        qt = ABa.get([128, 512]); kt = ABa.get([128, 512]); kh = ABa.get([128, 512])
        Qblk_r = ABa.ring("Qblk", [128, 4, 128], 4); Vblk_r = [ABa.get([128, 4, 512]) for _ in range(2)]; khT = ABa.get([128, 4, 128])
        ATm = ABa.get([128, 4, 128]); STm = ABa.get([128, 4, 128]); kw = ABa.get([128, 4, 128])
        Cb = ABa.get([128, 4, 130]); Sb = ABa.get([128, 4, 8, 128])
        yb16 = ABa.get([128, D]); yT = ABa.get([128, 8, 128]); h2_r = ABa.ring("h2", [128, D], 2)
        h2T = ABa.get([128, 8, 128]); junk = ABa.get([128, 128]); selb = ABa.get([128, 32])
        ss1 = sm[:, 0:1]; ss2 = sm[:, 1:2]; gt = sm[:, 8:16]; e4 = sm[:, 16:20]; l4 = sm[:, 20:24]
        tmpa = sm[:, 24:32]; aw = sm[:, 32:40]; ebdec = sm[:, 40:48]; ebc = sm[:, 48:52]; dn = sm[:, 52:56]
        sc = sm[:, 56:60]; ssm = sm[:, 60:64]; ssh = sm[:, 64:68]; decs = sm[:, 96:112]
        gmax = sm[:, 68:69]; ngmax = sm[:, 69:70]; ge = sm[:, 72:76]; gsum = sm[:, 76:77]; gval = sm[:, 77:78]
        G4 = sm[:, 80:84]; pen = sm[:, 84:88]; dd = sm[:, 88:89]; e2 = sm[:, 89:90]; w1 = sm[:, 90:91]
        d1f = sm[:, 91:92]; d2f = sm[:, 92:93]
        for q_ in range(2):
            V("pool", "memset", [], [("vext", q_)], ap=vext_r[q_][:, :, 128:130], constant=1.0)
        V("pool", "memset", [], ["Cb"], ap=Cb[:, :, :], constant=0.0)

        import os
        STOP = os.environ.get("K_STOP", "")

        class StopBuild(Exception):
            pass

        def stop_at(tag):
            if STOP == tag:
                raise StopBuild()

        WiK = [("Wi", k) for k in range(8)]
        state = {}

        def A_pre(i):
            xt, kx = xt_r.next()
            LD("sp", xt, x_d[i * 128:(i + 1) * 128, :], [kx])
            ACTV([kx], ["hb", "ss1"], out=hb, in_=xt, func=AF.Square, accum_out=ss1)
            rstd_from_ss(ss1, "ss1", D, 1)
            V("dve", "scalar_tensor_tensor", [kx, "ss1", "gA"], ["hb"], out=hb, in0=xt, scalar=ss1, in1=gA[:],
              op0=ALU.mult, op1=ALU.mult)
            hT, khT = hT_r.next()
            transpose8(hb, "hb", hT, khT, "act")
            state[i] = (xt, kx, hT, khT)

        FMCOL = {"q": 0, "k": 512, "hq": 2056, "hf": 2568}
        TMCOL = {"v": 1024, "o": 1536, "hv": 3080, "hg": 3592}

        def A_grp(i, name):
            xt, kx, hT, khT = state[i]
            q_ = i % 2
            rd = WiK + [khT]
            B, kb = pb.next()
            if name in FMCOL:
                col0 = FMCOL[name]
                for c in range(4):
                    MM(B[:, c * 128:(c + 1) * 128],
                       [(Wi3[:, k, col0 + c * 128:col0 + (c + 1) * 128], hT[:, k, :]) for k in range(8)], rd, [kb])
                B3 = B[:, :].rearrange("p (c t) -> p c t", c=4)
                if name == "q":
                    S.op("act", lambda e, B3=B3: e.copy(out=zqk[:, 0:4, 3:131], in_=B3), [kb], ["zqk"])
                elif name == "k":
                    S.op("act", lambda e, B3=B3: e.copy(out=zqk[:, 4:8, 3:131], in_=B3), [kb], ["zqk"])
                elif name == "hq":
                    S.op("act", lambda e, B=B: e.copy(out=qs, in_=B[:, :]), [kb], ["qs"])
                else:
                    ACTV([kb], ["sgf"], out=sgf, in_=B[:, :], func=AF.Exp, scale=-1.0)
                    V("dve", "tensor_scalar", ["sgf"], ["sgf"], out=sgf, in0=sgf, scalar1=1.0, scalar2=None, op0=ALU.add)
                    V("dve", "reciprocal", ["sgf"], ["sgf"], out=sgf, in_=sgf)
            elif name in TMCOL:
                col0 = TMCOL[name]
                MM(B[:, 0:512], [(hT[:, k, :], Wi3[:, k, col0:col0 + 512]) for k in range(8)], rd, [kb])
                if name == "v":
                    vx = vext_r[q_]
                    S.op("act", lambda e, B=B, vx=vx: e.copy(out=vx[:, :, 0:128], in_=B[:, :].rearrange("p (h e) -> p h e", h=4)),
                         [kb], [("vext", q_)])
                elif name == "o":
                    ACTV([kb], [("sig_o", q_)], out=sig_o_r[q_], in_=B[:, :], func=AF.Exp, scale=-1.0)
                    V("dve", "tensor_scalar", [("sig_o", q_)], [("sig_o", q_)], out=sig_o_r[q_], in0=sig_o_r[q_], scalar1=1.0, scalar2=None, op0=ALU.add)
                    V("dve", "reciprocal", [("sig_o", q_)], [("sig_o", q_)], out=sig_o_r[q_], in_=sig_o_r[q_])
                elif name == "hv":
                    hv_ = hvb_r[q_]; vb_ = Vblk_r[q_]
                    S.op("act", lambda e, B=B, hv_=hv_: e.copy(out=hv_, in_=B[:, :]), [kb], [("hvb", q_)])
                    for c in range(4):
                        ACTV([kb, "ms"], [("Vblk", q_)], out=vb_[:, c, :], in_=B[:, :], func=AF.Copy, scale=ms[:, c:c + 1])
                else:
                    g_ = gs_r[q_]
                    S.op("act", lambda e, B=B, g_=g_: e.copy(out=g_, in_=B[:, :]), [kb], [("gs", q_)])
            else:
                MM(B[:, 0:8], [(hT[:, k, :], Wi3[:, k, 2048:2056]) for k in range(8)], rd, [kb])
                V("dve", "tensor_tensor", [kb, "bg_bc"], ["gt"], out=gt, in0=B[:, 0:8], in1=bg_bc[:], op=ALU.add)

        def conv_silu(i):
            if i % TPS == 0:
                V("pool", "memset", [], ["zqk"], ap=zqk[:, :, 0:3], constant=0.0)
            else:
                V("pool", "tensor_copy", ["tailb"], ["zqk"], out=zqk[:, :, 0:3], in_=tailb)
            for j in range(4):
                wj = cw[:, :, j:j + 1].to_broadcast([128, 8, 128])
                if j == 0:
                    V("dve", "tensor_tensor", ["zqk", "cw"], ["acc"], out=acc, in0=zqk[:, :, 0:128], in1=wj, op=ALU.mult)
                else:
                    V("dve", "tensor_tensor", ["zqk", "cw"], ["lfb", "kk"], out=ctmp, in0=zqk[:, :, j:j + 128], in1=wj, op=ALU.mult)
                    V("dve", "tensor_tensor", ["acc", "lfb", "kk"], ["acc"], out=acc, in0=acc, in1=ctmp, op=ALU.add)
            V("pool", "tensor_copy", ["zqk"], ["tailb"], out=tailb, in_=zqk[:, :, 128:131])

        AORDER = (("v", "o"), ("hv", "hg"), ("q", "k"), ("hq", "hf", "gates"))

        def stageB(i, hook=lambda n: None, part="all"):
            q_ = i % 2
            vext = vext_r[q_]; sig_o = sig_o_r[q_]; hvb = hvb_r[q_]; Vblk = Vblk_r[q_]; gs = gs_r[q_]
            Kv, Kso, Khv, Kvb, Kgs = ("vext", q_), ("sig_o", q_), ("hvb", q_), ("Vblk", q_), ("gs", q_)
            seq_start = (i % TPS == 0)
            par = i % 2
            if part != "main":
                ACTV(["acc"], ["qkT"], out=qkT, in_=acc, func=AF.Silu)
                ACTV(["qs"], ["qs"], out=qs, in_=qs, func=AF.Silu)
                ACTV([Kgs], [Kgs], out=gs, in_=gs, func=AF.Silu)
                V("dve", "tensor_tensor", [Kgs, "gh_bc"], [Kgs], out=gs.rearrange("p (h e) -> p h e", h=4),
                  in0=gs.rearrange("p (h e) -> p h e", h=4),
                  in1=gh_bc[:, :].unsqueeze(1).to_broadcast([128, 4, 128]), op=ALU.mult)
                if seq_start:
                    V("pool", "memset", [], ["Cst"], ap=Cst[:, :, :], constant=0.0)
                    V("pool", "memset", ["Cb"], ["Cb"], ap=Cb[:, :, 0:129], constant=0.0)
                    V("pool", "memset", [], [("Sf", h) for h in range(4)], ap=Sf[:, :, :], constant=0.0)
                    V("pool", "memset", [], [("Sb", h) for h in range(4)], ap=Sb[:, :, (1 - par) * 4 + 3, :], constant=0.0)
                stop_at("B0")
                ACTV(["gt"], ["e4"], out=e4, in_=gt[:, 4:8], func=AF.Exp, scale=-1.0)
                ACTV(["e4"], ["l4"], out=l4, in_=e4, func=AF.Ln, bias=1.0)
                GP, kgp = pb.next()
                MM(GP[:, 0:4], [(tri[:], l4)], ["tri", "l4"], [kgp])
                MM(GP[:, 4:8], [(onesf[:], l4)], ["onesf", "l4"], [kgp])
                V("dve", "tensor_tensor", [kgp, "gt"], ["tmpa"], out=tmpa[:, 0:4], in0=GP[:, 0:4], in1=gt[:, 0:4], op=ALU.add)
                V("dve", "tensor_tensor", [kgp, "tmpa"], ["tmpa"], out=tmpa[:, 4:8], in0=tmpa[:, 0:4], in1=GP[:, 4:8], op=ALU.subtract)
                ACTV(["tmpa"], ["aw"], out=aw, in_=tmpa, func=AF.Exp)
                ACTV([kgp], ["ebdec"], out=ebdec, in_=GP[:, 0:8], func=AF.Exp, scale=-1.0)
                V("dve", "tensor_scalar", ["ebdec"], ["ebc"], out=ebc, in0=ebdec[:, 0:4], scalar1=float(128 ** -0.5), scalar2=None,
                  op0=ALU.mult)
                stop_at("B1")
                for h in range(4):
                    V("dve", "tensor_scalar", ["sgf", "lbp"], ["sgf"], out=sgf[:, h * 128:(h + 1) * 128], in0=sgf[:, h * 128:(h + 1) * 128],
                      scalar1=lbp[:, h, 1:2], scalar2=lbp[:, h, 0:1], op0=ALU.mult, op1=ALU.add)
                ACTV(["sgf"], ["lfb"], out=lfb, in_=sgf, func=AF.Ln)
                V("pool", "tensor_scalar", ["sgf"], ["kk"], out=kk, in0=sgf, scalar1=-1.0, scalar2=1.0, op0=ALU.mult, op1=ALU.add)
                V("dve", "tensor_tensor_scan", ["rmask", "lfb"], ["bb"], out=bb, data0=rmask[:], data1=lfb, initial=0.0,
                  op0=ALU.mult, op1=ALU.add)
                ACTV(["bb", "kk", "lfb"], ["sgf"], out=sgf, in_=bb, func=AF.Exp)
                ACTV(["bb"], ["lfb"], out=lfb, in_=bb, func=AF.Exp, scale=-1.0)
                V("dve", "tensor_tensor", ["qs", "sgf"], ["qt"], out=qt, in0=qs, in1=sgf, op=ALU.mult)
                V("dve", "tensor_tensor", ["kk", "lfb"], ["kt"], out=kt, in0=kk, in1=lfb, op=ALU.mult)
                V("dve", "tensor_tensor", ["kt", "sgf"], ["kh"], out=kh.rearrange("p (g s) -> p g s", s=32),
                  in0=kt.rearrange("p (g s) -> p g s", s=32),
                  in1=sgf.rearrange("p (g s) -> p g s", s=32)[:, :, 31:32].to_broadcast([128, 16, 32]), op=ALU.mult)
                stop_at("B2")
            if part == "pro":
                return
            hook(0)
            H4 = lambda ap: ap.rearrange("p (h t) -> p h t", h=4)
            STb, kst = pb.next()
            for h in range(4):
                MM(STb[:, h * 128:(h + 1) * 128], [(qkT[:, 4 + h, :], qkT[:, h, :])], ["qkT"], [kst])
            KTb, kkt = pt.next()
            for h in range(4):
                TR(KTb[:, h * 128:(h + 1) * 128], qkT[:, 4 + h, :], ["qkT"], [kkt], sig=(h == 3))
            ATb, kat = pb.next()
            for h in range(4):
                MM(ATb[:, h * 128:(h + 1) * 128], [(kt[:, h * 128:(h + 1) * 128], qt[:, h * 128:(h + 1) * 128])], ["kt", "qt"], [kat])
            KHb, kkh = pt.next()
            for h in range(4):
                TR(KHb[:, h * 128:(h + 1) * 128], kh[:, h * 128:(h + 1) * 128], ["kh"], [kkh], sig=(h == 3))
            for h in range(4):
                V("dve", "scalar_tensor_tensor", [kst, "aw", "tri"], [("STm", h)], out=STm[:, h, :], in0=STb[:, h * 128:(h + 1) * 128],
                  scalar=aw[:, h:h + 1], in1=tri[:], op0=ALU.mult, op1=ALU.mult)
            for h in range(4):
                ACTV([kkt, "aw"], [("kw", h)], out=kw[:, h, :], in_=KTb[:, h * 128:(h + 1) * 128], func=AF.Copy, scale=aw[:, 4 + h:5 + h])
            S.op("act", lambda e, KHb=KHb: e.copy(out=khT, in_=H4(KHb[:, 0:512])), [kkh], ["khT"])
            V("dve", "tensor_tensor", [kat, "blk"], ["ATm"], out=ATm, in0=H4(ATb[:, :]),
              in1=blk[:, :].unsqueeze(1).to_broadcast([128, 4, 128]), op=ALU.mult)
            Qbs = []
            for h in range(4):
                Qb, kqb = Qblk_r.next()
                V("dve", "tensor_tensor", ["qt", "mt"], [kqb], out=Qb,
                  in0=qt[:, h * 128:(h + 1) * 128].unsqueeze(1).to_broadcast([128, 4, 128]), in1=mt[:], op=ALU.mult)
                Qbs.append((Qb, kqb))
            V("dve", "tensor_copy", ["sgf"], ["decs"], out=decs, in_=sgf.rearrange("p (g s) -> p g s", s=32)[:, :, 31])
            hook(1)
            PPs = []
            for h in range(4):
                PP, kpp = pb.next()
                for c in range(4):
                    MM(PP[:, c * 128:(c + 1) * 128], [(khT[:, h, :], Vblk[:, c, h * 128:(h + 1) * 128])], ["khT", Kvb], [kpp])
                PPs.append((PP, kpp))
            for c in range(4):
                for h in range(4):
                    PP, kpp = PPs[h]
                    ksf, ksb = ("Sf", h), ("Sb", h)
                    V("dve", "scalar_tensor_tensor", [ksf, "decs", kpp], [ksf], out=Sf[:, h, :], in0=Sf[:, h, :],
                      scalar=decs[:, h * 4 + c:h * 4 + c + 1], in1=PP[:, c * 128:(c + 1) * 128], op0=ALU.mult, op1=ALU.add)
                    S.op("act", lambda e, h=h, c=c: e.copy(out=Sb[:, h, par * 4 + c, :], in_=Sf[:, h, :]), [ksf], [ksb])
            NUs = []
            for hp in range(2):
                NU, knu = pb.next()
                for hh in range(2):
                    h = 2 * hp + hh
                    MM(NU[:, hh * 129:(hh + 1) * 129], [(STm[:, h, :], vext[:, h, 0:129]), (qkT[:, h, :], Cb[:, h, 0:129])],
                       [("STm", h), Kv, "qkT", "Cb"], [knu])
                NUs.append((NU, knu))
            for hp in range(2):
                NU, knu = NUs[hp]
                V("dve", "tensor_tensor", [knu, "ebc"], ["dn"], out=dn[:, 2 * hp:2 * hp + 2],
                  in0=NU[:, 0:258].rearrange("p (h e) -> p h e", h=2)[:, :, 128], in1=ebc[:, 2 * hp:2 * hp + 2], op=ALU.mult)
            V("dve", "tensor_tensor", ["dn"], ["dn"], out=dn, in0=dn, in1=dn, op=ALU.mult)
            V("dve", "tensor_scalar", ["dn"], ["dn"], out=dn, in0=dn, scalar1=1.0, scalar2=None, op0=ALU.max)
            ACTV(["dn"], ["dn"], out=dn, in_=dn, func=AF.Ln)
            ACTV(["dn"], ["dn"], out=dn, in_=dn, func=AF.Exp, scale=-0.5)
            V("dve", "tensor_tensor", ["dn", "ebc"], ["sc"], out=sc, in0=dn, in1=ebc, op=ALU.mult)
            for h in range(4):
                NU, knu = NUs[h // 2]
                o_ = (h % 2) * 129
                V("dve", "scalar_tensor_tensor", [knu, "sc", Kso], ["hm"], out=hm[:, h, :], in0=NU[:, o_:o_ + 128],
                  scalar=sc[:, h:h + 1], in1=sig_o[:, h * 128:(h + 1) * 128], op0=ALU.mult, op1=ALU.mult)
                ACTV(["hm"], ["junk", "ssm"], out=junk[:, 0:128], in_=hm[:, h, :], func=AF.Square, accum_out=ssm[:, h:h + 1])
            hook(2)
            CUs = []
            for hp in range(2):
                CU, kcu = pb.next()
                for hh in range(2):
                    h = 2 * hp + hh
                    MM(CU[:, hh * 129:(hh + 1) * 129], [(kw[:, h, :], vext[:, h, 0:129])], [("kw", h), Kv], [kcu])
                CUs.append((CU, kcu))
            OO, koo = pb.next()
            for h in range(4):
                Qb, kqb = Qbs[h]
                pairs = [(ATm[:, h, :], hvb[:, h * 128:(h + 1) * 128]), (Qb[:, 0, :], Sb[:, h, (1 - par) * 4 + 3, :])]
                pairs += [(Qb[:, c, :], Sb[:, h, par * 4 + c - 1, :]) for c in range(1, 4)]
                MM(OO[:, h * 128:(h + 1) * 128], pairs, ["ATm", Khv, kqb, ("Sb", h)], [koo])
            for hp in range(2):
                CU, kcu = CUs[hp]
                for hh in range(2):
                    h = 2 * hp + hh
                    V("dve", "scalar_tensor_tensor", ["Cst", "ebdec", kcu], ["Cst"], out=Cst[:, h, :], in0=Cst[:, h, :],
                      scalar=ebdec[:, 4 + h:5 + h], in1=CU[:, hh * 129:(hh + 1) * 129], op0=ALU.mult, op1=ALU.add)
            S.op("act", lambda e: e.copy(out=Cb[:, :, 0:129], in_=Cst), ["Cst"], ["Cb"])
            for h in range(4):
                ACTV([koo], ["junk", "ssh"], out=junk[:, 0:128], in_=OO[:, h * 128:(h + 1) * 128], func=AF.Square,
                     accum_out=ssh[:, h:h + 1])
            V("dve", "tensor_tensor", [koo, Kgs], ["og"], out=og, in0=OO[:, :], in1=gs, op=ALU.mult)
            hook(3)
            rstd_from_ss(ssm, "ssm", 128, 4)
            rstd_from_ss(ssh, "ssh", 128, 4)
            for h in range(4):
                V("dve", "scalar_tensor_tensor", ["hm", "ssm", "gm_bc"], ["yb16"], out=yb16[:, h * 128:(h + 1) * 128],
                  in0=hm[:, h, :], scalar=ssm[:, h:h + 1], in1=gm_bc[:, h * 128:(h + 1) * 128], op0=ALU.mult, op1=ALU.mult)
            V("dve", "tensor_tensor", ["og", "ssh"], ["yb16"], out=H4(yb16[:, 512:1024]), in0=H4(og),
              in1=ssh.unsqueeze(2).to_broadcast([128, 4, 128]), op=ALU.mult)

        def stageC(i):
            xt, kx, hT, khT = state.pop(i)
            if dbg:
                yf = AFt[:, NF - 1024:NF]
                V("dve", "tensor_copy", ["yb16"], ["dbgy"], out=yf, in_=yb16)
                S.dma("sp", lambda e: e.dma_start(out=dbg_d["d_y"][i * 128:(i + 1) * 128, :], in_=yf), ["dbgy"], [], semkey=("dma", "dbgy"), final=True)
            transpose8(yb16, "yb16", yT, "yT", "act")
            for half in range(2):
                B, kb = pb.next()
                MM(B[:, 0:512], [(yT[:, k, :], Wo3[:, k, half * 512:(half + 1) * 512]) for k in range(8)], ["yT", "Wo"], [kb])
                V("dve", "tensor_tensor", [kb, kx], [kx], out=xt[:, half * 512:(half + 1) * 512], in0=B[:, 0:512],
                  in1=xt[:, half * 512:(half + 1) * 512], op=ALU.add)
            S.dma("sp", lambda e: e.dma_start(out=x1s[i * 128:(i + 1) * 128, :], in_=xt), [kx], [], semkey=("dma", "st", kx))
            if dbg:
                S.dma("sp", lambda e: e.dma_start(out=dbg_d["d_x1"][i * 128:(i + 1) * 128, :], in_=xt), [kx], [], semkey=("dma", "dbgx", kx), final=True)
            h2, kh2 = h2_r.next()
            ACTV([kx], [kh2, "ss2"], out=h2, in_=xt, func=AF.Square, accum_out=ss2)
            rstd_from_ss(ss2, "ss2", D, 1)
            V("dve", "scalar_tensor_tensor", [kx, "ss2", "gB"], [kh2], out=h2, in0=xt, scalar=ss2, in1=gB[:],
              op0=ALU.mult, op1=ALU.mult)
            transpose8(h2, kh2, h2T, "h2T", "act")
            S.call(lambda: S.pump(10 ** 9))
            ro = (i % 2) * 256
            RL = rbank[:, ro:ro + 64]; krl = ("pb", "r", i % 2)
            RK = rbank[:, ro + 64:ro + 128]; krk = krl
            MM(RL[:, 0:36], [(h2T[:, k, :], Wr[:, k, :]) for k in range(8)], ["h2T", "Wr"], [krl])
            _cap = S.capture()
            _lst = _cap.__enter__()
            V("dve", "tensor_tensor", [krl, "brt_bc"], ["lg"], out=lg, in0=RL[:, 0:36], in1=brt_bc[:], op=ALU.add)
            if dbg:
                S.dma("sp", lambda e: e.dma_start(out=dbg_d["d_lg"][i * 128:(i + 1) * 128, :], in_=lg), ["lg"], [], semkey=("dma", "dbglg"), final=True)
            V("dve", "tensor_reduce", ["lg"], ["gmax"], out=gmax, in_=lg[:, 0:4], axis=AX.X, op=ALU.max)
            V("dve", "tensor_scalar", ["gmax"], ["ngmax"], out=ngmax, in0=gmax, scalar1=-1.0, scalar2=None, op0=ALU.mult)
            ACTV(["lg", "ngmax"], ["ge", "gsum"], out=ge, in_=lg[:, 0:4], func=AF.Exp, bias=ngmax, scale=1.0, accum_out=gsum)
            V("dve", "reciprocal", ["gsum"], ["gval"], out=gval, in_=gsum)
            V("dve", "tensor_scalar", ["lg", "gmax"], ["G4"], out=G4, in0=lg[:, 0:4], scalar1=gmax, scalar2=None, op0=ALU.is_equal)
            V("dve", "tensor_scalar", ["G4"], ["pen"], out=pen, in0=G4, scalar1=1e30, scalar2=-1e30, op0=ALU.mult, op1=ALU.add)
            V("dve", "tensor_tensor", ["lg", "pen"], ["mskt"], out=mskt.rearrange("p (g j) -> p g j", g=4),
              in0=lg[:, 4:36].rearrange("p (g j) -> p g j", g=4), in1=pen.unsqueeze(2).to_broadcast([128, 4, 8]), op=ALU.add)
            V("dve", "max", ["mskt"], ["top8"], out=top8, in_=mskt)
            V("dve", "tensor_scalar", ["mskt", "top8"], ["sel1"], out=sel1, in0=mskt, scalar1=top8[:, 0:1], scalar2=None, op0=ALU.is_equal)
            V("dve", "tensor_scalar", ["mskt", "top8"], ["sel2"], out=sel2, in0=mskt, scalar1=top8[:, 1:2], scalar2=None, op0=ALU.is_equal)
            V("dve", "tensor_tensor", ["sel1", "sel2"], ["selb"], out=selb, in0=sel1, in1=sel2, op=ALU.add)
            V("dve", "tensor_tensor", ["top8"], ["dd"], out=dd, in0=top8[:, 1:2], in1=top8[:, 0:1], op=ALU.subtract)
            ACTV(["dd"], ["e2"], out=e2, in_=dd, func=AF.Exp)
            V("dve", "tensor_scalar", ["e2"], ["w1"], out=w1, in0=e2, scalar1=1.0, scalar2=None, op0=ALU.add)
            V("dve", "reciprocal", ["w1"], ["w1"], out=w1, in_=w1)
            V("dve", "tensor_tensor", ["w1", "gval"], ["cw1"], out=cw1[:, i:i + 1], in0=w1, in1=gval, op=ALU.mult)
            V("dve", "tensor_tensor", ["cw1", "e2"], ["cw2"], out=cw2[:, i:i + 1], in0=cw1[:, i:i + 1], in1=e2, op=ALU.mult)
            MM(RK[:, 0:32], [(stri[:], selb)], ["stri", "selb"], [krk])
            MM(RK[:, 32:64], [(onesb[:], selb)], ["onesb", "selb"], [krk])
            V("dve", "tensor_tensor", [krk, "base"], ["pos"], out=pos, in0=RK[:, 0:32], in1=base[:], op=ALU.add)
            V("dve", "tensor_tensor", [krk, "base"], ["base"], out=base[:], in0=RK[:, 32:64], in1=base[:], op=ALU.add)
            V("dve", "tensor_scalar", ["pos"], ["tmp32"], out=tmp32, in0=pos, scalar1=float(CAP), scalar2=1e9, op0=ALU.is_ge, op1=ALU.mult)
            V("dve", "tensor_tensor", ["pos", "ec"], ["pos"], out=pos, in0=pos, in1=ec[:], op=ALU.add)
            V("dve", "tensor_tensor", ["pos", "tmp32"], ["pos"], out=pos, in0=pos, in1=tmp32, op=ALU.add)
            V("dve", "tensor_scalar", ["pos"], ["pos"], out=pos, in0=pos, scalar1=float(ZROW), scalar2=None, op0=ALU.min)
            V("dve", "tensor_tensor", ["pos", "sel1"], ["sel1"], out=sel1, in0=sel1, in1=pos, op=ALU.mult)
            V("dve", "tensor_tensor", ["pos", "sel2"], ["sel2"], out=sel2, in0=sel2, in1=pos, op=ALU.mult)
            V("dve", "tensor_reduce", ["sel1"], ["d1f"], out=d1f, in_=sel1, axis=AX.X, op=ALU.add)
            V("dve", "tensor_reduce", ["sel2"], ["d2f"], out=d2f, in_=sel2, axis=AX.X, op=ALU.add)
            V("dve", "tensor_copy", ["d1f"], [("d1i", i)], out=d1i[:, i:i + 1], in_=d1f)
            V("dve", "tensor_copy", ["d2f"], [("d2i", i)], out=d2i[:, i:i + 1], in_=d2f)
            if dbg:
                sl = AFt[:, NF - 1028:NF - 1024]
                V("dve", "tensor_copy", ["d1f"], ["dbgs"], out=sl[:, 0:1], in_=d1f)
                V("dve", "tensor_copy", ["d2f"], ["dbgs"], out=sl[:, 1:2], in_=d2f)
                V("dve", "tensor_copy", ["cw1"], ["dbgs"], out=sl[:, 2:3], in_=cw1[:, i:i + 1])
                V("dve", "tensor_copy", ["cw2"], ["dbgs"], out=sl[:, 3:4], in_=cw2[:, i:i + 1])
                S.dma("sp", lambda e: e.dma_start(out=dbg_d["d_slot"][i * 128:(i + 1) * 128, :], in_=sl), ["dbgs"], [], semkey=("dma", "dbgs"), final=True)
            for di, kd in ((d1i, ("d1i", i)), (d2i, ("d2i", i))):
                S.dma("pool", lambda e, di=di, h2=h2: e.indirect_dma_start(
                    out=xs_d, out_offset=bass.IndirectOffsetOnAxis(ap=di[:, i:i + 1], axis=0), in_=h2, in_offset=None),
                    [kh2, kd], [], semkey=("dma", "scat", kh2, kd[0]))
            _cap.__exit__(None, None, None)
            S.call(lambda _lst=_lst: S.deferred.extend(_lst))

        try:
            A_pre(0)
            for grp in AORDER:
                for nm in grp:
                    A_grp(0, nm)
            conv_silu(0)
            stageB(0, part="pro")

            def hook_for(i):
                def hk(n):
                    for nm in AORDER[n]:
                        A_grp(i + 1, nm)
                    if n == 2:
                        conv_silu(i + 1)
                return hk

            def zip2(la, lb):
                na, nb = len(la), len(lb)
                ia = ib = 0
                while ia < na or ib < nb:
                    if ib >= nb or (ia < na and ia * nb <= ib * na):
                        la[ia](); ia += 1
                    else:
                        lb[ib](); ib += 1
            for i in range(NT):
                if i + 1 < NT:
                    A_pre(i + 1)
                    hk = hook_for(i)
                else:
                    hk = lambda n: None
                stageB(i, hk, part="main")
                with S.capture() as lc_:
                    stageC(i)
                lp_ = []
                if i + 1 < NT:
                    with S.capture() as lp_:
                        stageB(i + 1, part="pro")
                zip2(lc_, lp_)
        except StopBuild:
            pass
        if STOP in ("A", "B", "C", "P1"):
            S.pump(10 ** 9)
            S.flush()
            return nc

        S.barrier()
        AFa.reset(); ABa.reset()
        NWE = 8 * 512
        wslots = []
        for s in range(2):
            o = s * 3 * NWE
            wslots.append((Wi[:, o:o + NWE].rearrange("p (k n) -> p k n", k=8),
                           Wi[:, o + NWE:o + 2 * NWE].rearrange("p (k n) -> p k n", k=8),
                           Wi[:, o + 2 * NWE:o + 3 * NWE].rearrange("p (f n) -> p f n", f=4)))
        Wpg = Wi[:, 6 * NWE:6 * NWE + 8 * D].rearrange("p (k n) -> p k n", k=8)
        Wpp = Wo[:, 0:2 * D].rearrange("p (k n) -> p k n", k=2)
        xr_r = ABa.ring("xr", [128, NB, D], 2); xT_r = ABa.ring("xT", [128, 8, CAP], 2); act_r = ABa.ring("actT", [128, 4, CAP], 2)
        yo_r = AFa.ring("yo", [128, D], 2); sgt_r = AFa.ring("sgt", [128, CAP], 2); stg_r = AFa.ring("stg", [128, 4096], 2)

        wsrc = lambda e: (("g", weg[e].rearrange("(p k) n -> p k n", k=8), 8),
                          ("u", weu[e].rearrange("(p k) n -> p k n", k=8), 8),
                          ("d", wed[e].rearrange("(p f) n -> p f n", f=4), 4))
        staged = {}

        def stage_load(e, j):
            nm, src, kk_ = wsrc(e)[j]
            st, kst = stg_r.next()
            st3 = st.rearrange("p (k n) -> p k n", k=kk_)
            LD("sp", st3, src, [kst])
            staged[(e, j)] = (st3, kst, kk_, nm)

        def cast_w(e, j):
            st3, kst, kk_, nm = staged.pop((e, j))
            dst = wslots[e % 2][j]
            kq = ("wexp", e % 2)
            h_ = kk_ // 2
            S.op("act", lambda en, dst=dst, st3=st3, h_=h_: en.copy(out=dst[:, 0:h_, :], in_=st3[:, 0:h_, :]), [kst], [kq + (nm, 0)])
            V("dve", "tensor_copy", [kst], [kq + (nm, 1)], out=dst[:, h_:, :], in_=st3[:, h_:, :])

        def load_expert(e):
            stage_load(e, 0); stage_load(e, 1); cast_w(e, 0); stage_load(e, 2); cast_w(e, 1); cast_w(e, 2)

        load_expert(0)
        stage_load(1, 0); stage_load(1, 1)
        LD("pool", Wpg, wplg.rearrange("(k p) n -> p k n", p=128), ["Wpg"])
        LD("pool", Wpp, wplp.rearrange("(k p) n -> p k n", p=128), ["Wpp"])
        LD("sp", gA[:], g_pl.partition_broadcast(128), ["gA"])
        LD("sp", gB[:], g_fin.partition_broadcast(128), ["gB"])
        for e in range(NE):
            wg, wu, wd = wslots[e % 2]
            kq = ("wexp", e % 2)
            xr, kxr = xr_r.next()
            LD("pool", xr, xs_d[e * CAP:(e + 1) * CAP, :].rearrange("(b p) d -> p b d", p=128), [kxr])
            xT, kxT = xT_r.next()
            for b in range(NB):
                P, kp = pt.next()
                for k in range(8):
                    TR(P[:, k * 128:(k + 1) * 128], xr[:, b, k:D:8], [kxr], [kp], sig=(k == 7))
                if b % 2 == 0:
                    S.op("act", lambda en, P=P, b=b, xT=xT: en.copy(out=xT[:, :, b * 128:(b + 1) * 128],
                                                                   in_=P[:, :].rearrange("p (k t) -> p k t", k=8)), [kp], [kxT])
                else:
                    V("dve", "tensor_copy", [kp], [kxT], out=xT[:, :, b * 128:(b + 1) * 128],
                      in_=P[:, :].rearrange("p (k t) -> p k t", k=8))
            if e + 1 < NE:
                cast_w(e + 1, 0); cast_w(e + 1, 1); stage_load(e + 1, 2)
            aT, kaT = act_r.next()
            for f in range(4):
                Gp, kg = pb.next()
                MM(Gp[:, 0:CAP], [(wg[:, k, f:DE:4], xT[:, k, :]) for k in range(8)], [kq + ("g", 0), kq + ("g", 1), kxT], [kg])
                Up, ku = pb.next()
                MM(Up[:, 0:CAP], [(wu[:, k, f:DE:4], xT[:, k, :]) for k in range(8)], [kq + ("u", 0), kq + ("u", 1), kxT], [ku])
                sg, ksg = sgt_r.next()
                ACTV([kg], [ksg], out=sg, in_=Gp[:, 0:CAP], func=AF.Silu)
                V("dve", "tensor_tensor", [ksg, ku], [kaT], out=aT[:, f, :], in0=sg, in1=Up[:, 0:CAP], op=ALU.mult)
            if e + 1 < NE:
                cast_w(e + 1, 2)
            if e + 2 < NE:
                stage_load(e + 2, 0); stage_load(e + 2, 1)
            for b in range(NB):
                yo, kyo = yo_r.next()
                for half in range(2):
                    Yp, ky = pb.next()
                    MM(Yp[:, 0:512], [(aT[:, f, b * 128:(b + 1) * 128], wd[:, f, half * 512:(half + 1) * 512]) for f in range(4)],
                       [kaT, kq + ("d", 0), kq + ("d", 1)], [ky])
                    if half == 0:
                        S.op("act", lambda en, yo=yo, Yp=Yp: en.copy(out=yo[:, 0:512], in_=Yp[:, 0:512]), [ky], [kyo])
                    else:
                        V("dve", "tensor_copy", [ky], [kyo], out=yo[:, 512:1024], in_=Yp[:, 0:512])
                r0 = e * CAP + b * 128
                S.dma("act", lambda en, yo=yo, r0=r0: en.dma_start(out=yb_d[r0:r0 + 128, :], in_=yo), [kyo], [], semkey=("dma", "st", kyo))

        if STOP == "P2":
            S.flush()
            return nc
        S.barrier()
        AFa.reset(); ABa.reset()
        x1_r = AFa.ring("x1", [128, D], 3); y1_r = AFa.ring("y1", [128, D], 2); y2_r = AFa.ring("y2", [128, D], 2)
        pf_r = AFa.ring("pf", [128, 256], 3); sg3_r = AFa.ring("sg3", [128, D], 1); ot_r = AFa.ring("ot", [128, D], 2)
        sm3_r = AFa.ring("sm3", [128, 8], 3)
        h3_r = ABa.ring("h3", [128, D], 2); h3T_r = ABa.ring("h3T", [128, 8, 128], 2); pbf_r = ABa.ring("pbf", [128, 256], 2)
        pT_r = ABa.ring("pT", [128, 2, 128], 2); junk3 = ABa.get([128, D])
        p3_ld = {}

        def p3_loads(i):
            x1, kx1 = x1_r.next(); pf, kpf = pf_r.next()
            LD("sp", x1, x1s[i * 128:(i + 1) * 128, :], [kx1])
            LD("sp", pf, p_d[i * 128:(i + 1) * 128, :], [kpf])
            p3_ld[i] = (x1, kx1, pf, kpf)

        def p3_front(i):
            y1, ky1 = y1_r.next(); y2, ky2 = y2_r.next()
            sg3, ksg3 = sg3_r.next(); sm3, ksm3 = sm3_r.next(); h3, kh3 = h3_r.next(); pbf, kpbf = pbf_r.next()
            ss3 = sm3[:, 0:1]; ss4 = sm3[:, 1:2]; kss3 = ksm3 + ("a",); kss4 = ksm3 + ("b",)
            if i == 0:
                p3_loads(0)
            if i + 1 < NT:
                p3_loads(i + 1)
            x1, kx1, pf, kpf = p3_ld.pop(i)
            for yy, kyy, di in ((y1, ky1, d1i), (y2, ky2, d2i)):
                S.dma("pool", lambda e, yy=yy, di=di, i=i: e.indirect_dma_start(
                    out=yy, out_offset=None, in_=yb_d, in_offset=bass.IndirectOffsetOnAxis(ap=di[:, i:i + 1], axis=0)),
                    ["yb_z"], [kyy])
            V("dve", "scalar_tensor_tensor", [kx1, ky1], [kx1], out=x1, in0=y1, scalar=cw1[:, i:i + 1], in1=x1, op0=ALU.mult, op1=ALU.add)
            V("dve", "scalar_tensor_tensor", [kx1, ky2], [kx1], out=x1, in0=y2, scalar=cw2[:, i:i + 1], in1=x1, op0=ALU.mult, op1=ALU.add)
            ACTV([kx1], ["junk3", kss3], out=junk3, in_=x1, func=AF.Square, accum_out=ss3)
            rstd_from_ss(ss3, kss3, D, 1)
            V("dve", "scalar_tensor_tensor", [kx1, kss3, "gA"], [kh3], out=h3, in0=x1, scalar=ss3, in1=gA[:], op0=ALU.mult, op1=ALU.mult)
            h3T, kh3T = h3T_r.next()
            transpose8(h3, kh3, h3T, kh3T, "act")
            S.op("act", lambda e, pf=pf, pbf=pbf: e.copy(out=pbf, in_=pf), [kpf], [kpbf])
            P, kp = pt.next()
            for k in range(2):
                TR(P[:, k * 128:(k + 1) * 128], pbf[:, k * 128:(k + 1) * 128], [kpbf], [kp], sig=(k == 1))
            pT, kpT = pT_r.next()
            V("dve", "tensor_copy", [kp], [kpT], out=pT, in_=P[:, 0:256].rearrange("p (k t) -> p k t", k=2))
            return dict(x1=x1, kx1=kx1, sg3=sg3, ksg3=ksg3, sm3=sm3, ksm3=ksm3, h3T=h3T, kh3T=kh3T, pT=pT, kpT=kpT)

        def p3_back(i, c_):
            x1, kx1, sg3, ksg3, sm3, ksm3 = c_['x1'], c_['kx1'], c_['sg3'], c_['ksg3'], c_['sm3'], c_['ksm3']
            h3T, kh3T, pT, kpT = c_['h3T'], c_['kh3T'], c_['pT'], c_['kpT']
            ss4 = sm3[:, 1:2]; kss4 = ksm3 + ('b',)
            for half in range(2):
                Gp, kg = pb.next()
                MM(Gp[:, 0:512], [(h3T[:, k, :], Wpg[:, k, half * 512:(half + 1) * 512]) for k in range(8)], [kh3T, "Wpg"], [kg])
                Pp, kpp = pb.next()
                MM(Pp[:, 0:512], [(pT[:, k, :], Wpp[:, k, half * 512:(half + 1) * 512]) for k in range(2)], [kpT, "Wpp"], [kpp])
                ACTV([kg], [ksg3], out=sg3[:, half * 512:(half + 1) * 512], in_=Gp[:, 0:512], func=AF.Exp, scale=-1.0)
                V("dve", "tensor_scalar", [ksg3], [ksg3], out=sg3[:, half * 512:(half + 1) * 512],
                  in0=sg3[:, half * 512:(half + 1) * 512], scalar1=1.0, scalar2=None, op0=ALU.add)
                V("dve", "reciprocal", [ksg3], [ksg3], out=sg3[:, half * 512:(half + 1) * 512], in_=sg3[:, half * 512:(half + 1) * 512])
                V("dve", "tensor_tensor", [ksg3, kpp], [ksg3], out=sg3[:, half * 512:(half + 1) * 512],
                  in0=sg3[:, half * 512:(half + 1) * 512], in1=Pp[:, 0:512], op=ALU.mult)
            V("dve", "tensor_tensor", [ksg3, kx1], [kx1], out=x1, in0=x1, in1=sg3, op=ALU.add)
            ACTV([kx1], ["junk3", kss4], out=junk3, in_=x1, func=AF.Square, accum_out=ss4)
            rstd_from_ss(ss4, kss4, D, 1)
            ot, kot = ot_r.next()
            V("dve", "scalar_tensor_tensor", [kx1, kss4, "gB"], [kot], out=ot, in0=x1, scalar=ss4, in1=gB[:], op0=ALU.mult, op1=ALU.mult)
            S.dma("sp", lambda e, ot=ot, i=i: e.dma_start(out=out_d[i * 128:(i + 1) * 128, :], in_=ot), [kot], [], semkey=("dma", "st", kot), final=True)

        def zip_emit(la, lb):
            na, nb = len(la), len(lb)
            ia = ib = 0
            while ia < na or ib < nb:
                if ib >= nb or (ia < na and ia * nb <= ib * na):
                    la[ia](); ia += 1
                else:
                    lb[ib](); ib += 1

        ctx3 = p3_front(0)
        for i in range(NT):
            nxt = None
            lf_ = []
            if i + 1 < NT:
                with S.capture() as lf_:
                    nxt = p3_front(i + 1)
            with S.capture() as lb_:
                p3_back(i, ctx3)
            zip_emit(lb_, lf_)
            ctx3 = nxt
        S.flush()
    return nc


def _consts(CAP):
    s = np.arange(128)
    tri = (s[:, None] <= s[None, :]).astype(np.float32)
    stri = (s[:, None] < s[None, :]).astype(np.float32)
    blk = tri * ((s[:, None] // 32) == (s[None, :] // 32))
    rmask = np.tile(((np.arange(512) % 32) != 0).astype(np.float32)[None, :], (128, 1))
    mt = np.zeros((128, 4, 128), np.float32)
    ms = np.zeros((128, 4), np.float32)
    for c in range(4):
        mt[:, c, c * 32:(c + 1) * 32] = 1.0
        ms[c * 32:(c + 1) * 32, c] = 1.0
    ec = np.tile((np.arange(32) * CAP).astype(np.float32)[None, :], (128, 1))
    return {"c_ident": np.eye(128, dtype=np.float32), "c_tri": tri, "c_blk": blk.astype(np.float32), "c_stri": stri,
            "c_rmask": rmask, "c_mt": mt, "c_ms": ms, "c_ec": ec}


def make_in_maps(inp, n_cores, seqs_per_core):
    f = lambda a: np.ascontiguousarray(np.asarray(a, dtype=np.float32))
    x = f(inp["x"]); p = f(inp["p"])[0]
    B, T, _ = x.shape
    shared = {
        "w_in": f(inp["w_in"])[0], "w_out": f(inp["w_out"])[0],
        "wrt": f(np.concatenate([inp["w_rg"][0], inp["w_re"][0]], axis=1)),
        "brt": f(np.concatenate([inp["b_rg"][0], inp["b_re"][0]], axis=0))[None, :],
        "cw": f(np.asarray(inp["conv_qk"])[0].T.reshape(8, 128, 4).transpose(1, 0, 2)),
        "hl": f(np.asarray(inp["hg_lb"]).reshape(2, 4, 128).transpose(2, 1, 0)),
        "g_mix": f(inp["g_mix"])[0:1], "g_ffn": f(inp["g_ffn"])[0:1], "g_pl": f(inp["g_pl"])[0:1],
        "g_final": f(inp["g_final"])[None, :], "g_mlstm": f(inp["g_mlstm"])[0:1], "g_hgrn": f(inp["g_hgrn"])[0:1],
        "b_mgate": f(inp["b_mgate"])[0:1],
        "w_e_gate": f(inp["w_e_gate"])[0], "w_e_up": f(inp["w_e_up"])[0], "w_e_down": f(inp["w_e_down"])[0],
        "w_pl_gate": f(inp["w_pl_gate"])[0], "w_pl_proj": f(inp["w_pl_proj"])[0],
    }
    maps = []
    for c in range(n_cores):
        b0 = c * seqs_per_core
        m = dict(shared)
        m["x"] = f(x[b0:b0 + seqs_per_core].reshape(seqs_per_core * T, D))
        m["p"] = f(p[b0:b0 + seqs_per_core].reshape(seqs_per_core * T, 256))
        maps.append(m)
    return maps


def run(inp, n_cores, seqs_per_core, CAP, dbg=False):
    x = np.asarray(inp["x"])
    B, T, _ = x.shape
    TPS = T // 128
    NT = TPS * seqs_per_core
    nc = build_nc(NT, TPS, CAP, dbg=dbg)
    maps = make_in_maps(inp, n_cores, seqs_per_core)
    cst = _consts(CAP)
    for m in maps:
        m.update(cst)
    res = run_bass_kernel_spmd(nc, maps, core_ids=list(range(n_cores)))
    out = np.concatenate([np.asarray(r["out"]).reshape(seqs_per_core, T, D) for r in res.results], axis=0)
    if dbg:
        return out.astype(np.float32), res.results
    return out.astype(np.float32)


def kernel(**inputs):
    return run(inputs, 8, 2, 512)
```

```python
import numpy as np
from contextlib import ExitStack
import concourse.bass as bass
import concourse.mybir as mybir
from concourse.bass_utils import run_bass_kernel_spmd

F32 = mybir.dt.float32
BF16 = mybir.dt.bfloat16
I32 = mybir.dt.int32
AF = mybir.ActivationFunctionType
ALU = mybir.AluOpType
AX = mybir.AxisListType

ENGS = ("pe", "act", "dve", "pool", "sp")
EPS = 1e-6
D = 1024
NE = 32
DE = 512


class Sched:
    def __init__(self, nc, es, n_dma_sems=80, strict_same=True):
        self.nc = nc
        self.ops = {e: [] for e in ENGS}
        self.cnt = {}
        self.sem = {}
        self.seen = {}
        self.res = {}
        self.strict_same = strict_same
        for e in ENGS:
            self.sem[e] = es.enter_context(nc.semaphore("s_" + e))
            self.cnt[e] = 0
        self.free_sems = [es.enter_context(nc.semaphore("d%d" % i)) for i in range(n_dma_sems)]
        self.final_waits = []
        self.cap = None
        self.deferred = []
        self.pumping = False

    def _r(self, key):
        r = self.res.get(key)
        if r is None:
            r = self.res[key] = {"w": None, "r": {}}
        return r

    def _deps(self, reads, writes):
        deps = []
        for k in reads:
            r = self._r(k)
            if r["w"] is not None:
                deps.append(r["w"])
            if isinstance(k, tuple) and k[0] in ("pb", "pt"):
                deps.extend(r["r"].items())
        for k in writes:
            r = self._r(k)
            if r["w"] is not None:
                deps.append(r["w"])
            deps.extend(r["r"].items())
        return deps

    def _waits(self, eng, deps):
        best = {}
        for (x, v) in deps:
            if x == eng and (not self.strict_same or eng == "pe"):
                continue
            if v > best.get(x, 0):
                best[x] = v
        out = []
        for x, v in best.items():
            if v > self.seen.get((eng, x), 0):
                self.seen[(eng, x)] = v
                out.append((self.sem[x], v))
        return out

    def _commit(self, stamp, reads, writes):
        x, v = stamp
        for k in reads:
            r = self._r(k)
            if r["r"].get(x, 0) < v:
                r["r"][x] = v
        for k in writes:
            r = self._r(k)
            r["w"] = (x, v)
            r["r"] = {}

    def op(self, eng, fn, reads=(), writes=(), sig=True):
        if self.cap is not None:
            self.cap.append(lambda: self.op(eng, fn, reads, writes, sig))
            return
        self._op(eng, fn, reads, writes, sig)
        if eng == "dve" and self.deferred and not self.pumping:
            self.pump(1)

    def pump(self, n):
        self.pumping = True
        while n > 0 and self.deferred:
            self.deferred.pop(0)()
            n -= 1
        self.pumping = False

    def capture(self):
        sch = self

        class _C:
            def __enter__(s_):
                s_.prev = sch.cap
                sch.cap = []
                s_.lst = sch.cap
                return s_.lst

            def __exit__(s_, *a):
                sch.cap = s_.prev
                return False
        return _C()

    def call(self, fn):
        if self.cap is not None:
            self.cap.append(fn)
        else:
            fn()

    def _op(self, eng, fn, reads=(), writes=(), sig=True):
        waits = self._waits(eng, self._deps(reads, writes))
        if sig:
            self.cnt[eng] += 1
            stamp = (eng, self.cnt[eng])
            inc = (self.sem[eng], 1)
        else:
            stamp = (eng, self.cnt[eng] + 1)
            inc = None
        self.ops[eng].append((waits, fn, inc))
        self._commit(stamp, reads, writes)

    def dma(self, q, fn, reads=(), writes=(), semkey=None, final=False):
        if self.cap is not None:
            self.cap.append(lambda: self.dma(q, fn, reads, writes, semkey, final))
            return
        if semkey is None:
            semkey = ("dma", (tuple(writes) + tuple(reads))[0])
        if semkey not in self.sem:
            self.sem[semkey] = self.free_sems.pop()
            self.cnt[semkey] = 0
        waits = self._waits(q, self._deps(reads, writes))
        self.cnt[semkey] += 16
        stamp = (semkey, self.cnt[semkey])
        self.ops[q].append((waits, fn, (self.sem[semkey], 16)))
        self._commit(stamp, reads, writes)
        if final:
            self.final_waits.append(stamp)

    def barrier(self):
        self.pump(10 ** 9)
        tot = [(x, v) for x, v in self.cnt.items() if v > 0]
        for e in ENGS:
            waits = self._waits(e, tot)
            if waits:
                self.ops[e].append((waits, None, None))
        self.res = {}

    def flush(self):
        nc = self.nc
        fin = [(self.sem[x], v) for x, v in self.cnt.items() if v > 0 and x != "sp"]
        ops = self.ops

        def run(engine, lst, tail=()):
            for (waits, fn, inc) in lst:
                for (s, v) in waits:
                    engine.wait_ge(s, v)
                if fn is None:
                    continue
                ins = fn(engine)
                if inc is not None:
                    ins.then_inc(inc[0], inc[1])
            for (s, v) in tail:
                engine.wait_ge(s, v)

        with nc.Block() as block:
            @block.sync
            def _(e):
                run(e, ops["sp"], fin)

            @block.scalar
            def _(e):
                run(e, ops["act"])

            @block.vector
            def _(e):
                run(e, ops["dve"])

            @block.gpsimd
            def _(e):
                run(e, ops["pool"])

            @block.tensor
            def _(e):
                run(e, ops["pe"])


class Ring:
    sched = None

    def __init__(self, name, aps):
        self.name, self.t, self.i = name, aps, -1

    def next(self):
        self.i = (self.i + 1) % len(self.t)
        key = (self.name, self.i)
        S_ = Ring.sched
        if S_ is not None and S_.cap is None and self.name in ("pb", "pt"):
            r = S_.res.get(key)
            assert r is None or r["w"] is None or len(r["r"]) > 0, ("PSUM ring slot reused before its content was read", key)
        return self.t[self.i], key


class Arena:
    def __init__(self, t, n):
        self.t, self.n, self.off = t, n, 0

    def reset(self):
        self.off = 0

    def get(self, shape):
        n = int(np.prod(shape[1:]))
        n_al = (n + 15) // 16 * 16
        assert self.off + n_al <= self.n, ("arena overflow", self.off, n_al, self.n)
        ap = self.t[:, self.off:self.off + n]
        self.off += n_al
        if len(shape) == 3:
            ap = ap.rearrange("p (a b) -> p a b", a=shape[1])
        elif len(shape) == 4:
            ap = ap.rearrange("p (a b c) -> p a b c", a=shape[1], b=shape[2])
        return ap

    def ring(self, name, shape, n):
        return Ring(name, [self.get(shape) for _ in range(n)])


def build_nc(NT, TPS, CAP, dbg=False):
    NTOK = NT * 128
    NB = CAP // 128
    ZROW = NE * CAP
    nc = bass.Bass("TRN2", target_bir_lowering=False)

    def din(name, shape, dt=F32):
        return nc.dram_tensor(name, list(shape), dt, kind="ExternalInput").ap()

    x_d = din("x", [NTOK, D]); p_d = din("p", [NTOK, 256])
    w_in = din("w_in", [D, 4104]); w_out = din("w_out", [D, D])
    wrt_d = din("wrt", [D, 36]); brt_d = din("brt", [1, 36])
    cw_d = din("cw", [128, 8, 4]); hl_d = din("hl", [128, 4, 2])
    g_mix = din("g_mix", [1, D]); g_ffn = din("g_ffn", [1, D]); g_pl = din("g_pl", [1, D]); g_fin = din("g_final", [1, D])
    g_ml = din("g_mlstm", [1, 512]); g_hg = din("g_hgrn", [1, 128]); bg_d = din("b_mgate", [1, 8])
    weg = din("w_e_gate", [NE, D, DE]); weu = din("w_e_up", [NE, D, DE]); wed = din("w_e_down", [NE, DE, D])
    wplg = din("w_pl_gate", [D, D]); wplp = din("w_pl_proj", [256, D])
    c_ident = din("c_ident", [128, 128]); c_tri = din("c_tri", [128, 128]); c_blk = din("c_blk", [128, 128])
    c_stri = din("c_stri", [128, 128]); c_rmask = din("c_rmask", [128, 512])
    c_mt = din("c_mt", [128, 4, 128]); c_ms = din("c_ms", [128, 4]); c_ec = din("c_ec", [128, 32])
    out_d = nc.dram_tensor("out", [NTOK, D], F32, kind="ExternalOutput").ap()
    x1s = nc.dram_tensor("x1s", [NTOK, D], F32, kind="Internal").ap()
    xs_d = nc.dram_tensor("xs", [ZROW + 1, D], BF16, kind="Internal").ap()
    yb_d = nc.dram_tensor("yb", [ZROW + 1, D], F32, kind="Internal").ap()
    dbg_d = {}
    if dbg:
        for nm, shp in (("d_y", [NTOK, D]), ("d_x1", [NTOK, D]), ("d_lg", [NTOK, 36]), ("d_slot", [NTOK, 4])):
            dbg_d[nm] = nc.dram_tensor(nm, shp, F32, kind="ExternalOutput").ap()

    with ExitStack() as es:
        S = Sched(nc, es)
        Ring.sched = S
        sbt = lambda name, shape, dt: es.enter_context(nc.sbuf_tensor("sb_" + name, shape, dt))
        Wi = sbt("Wi", [128, 8 * 4104], BF16)
        Wi3 = Wi[:, :].rearrange("p (k n) -> p k n", k=8)
        Wo = sbt("Wo", [128, 8 * D], BF16)
        Wo3 = Wo[:, :].rearrange("p (k n) -> p k n", k=8)
        Wr = sbt("Wr", [128, 8, 36], BF16)
        gA = sbt("gA", [128, D], F32)
        gB = sbt("gB", [128, D], F32)
        gm_bc = sbt("gm_bc", [128, 512], F32)
        gh_bc = sbt("gh_bc", [128, 128], F32)
        bg_bc = sbt("bg_bc", [128, 8], F32)
        brt_bc = sbt("brt_bc", [128, 36], F32)
        cw = sbt("cw", [128, 8, 4], F32)
        hl = sbt("hl", [128, 4, 2], F32)
        lbp = sbt("lbp", [128, 4, 2], F32)
        ident = sbt("ident", [128, 128], BF16)
        tri = sbt("tri", [128, 128], F32)
        onesf = sbt("onesf", [128, 128], F32)
        blk = sbt("blk", [128, 128], F32)
        stri = sbt("stri", [128, 128], BF16)
        onesb = sbt("onesb", [128, 128], BF16)
        rmask = sbt("rmask", [128, 512], F32)
        mt = sbt("mt", [128, 4, 128], BF16)
        ms = sbt("ms", [128, 4], F32)
        ec = sbt("ec", [128, 32], F32)
        d1i = sbt("d1i", [128, NT], I32); d2i = sbt("d2i", [128, NT], I32)
        cw1 = sbt("cw1", [128, NT], F32); cw2 = sbt("cw2", [128, NT], F32)
        base = sbt("base", [128, 32], F32)
        zrow = sbt("zrow", [128, 8], F32)
        NF, NBF = (12300 if dbg else 11300), 25900
        AFt = sbt("arenaF", [128, NF], F32); ABt = sbt("arenaB", [128, NBF], BF16)
        AFa, ABa = Arena(AFt, NF), Arena(ABt, NBF)
        pb = Ring("pb", [es.enter_context(nc.psum_tensor("pb%d" % i, [128, 512], F32)) for i in range(5)])
        rbank = es.enter_context(nc.psum_tensor("rbank", [128, 512], F32))
        pt = Ring("pt", [es.enter_context(nc.psum_tensor("pt%d" % i, [128, 1024], BF16)) for i in range(2)])

        def V(eng, name, reads, writes, **kw):
            S.op(eng, lambda e: getattr(e, name)(**kw), reads, writes)

        def ACTV(reads, writes, **kw):
            S.op("act", lambda e: e.activation(**kw), reads, writes)

        def MM(out, pairs, reads, writes):
            n = len(pairs)
            for i, (l, r) in enumerate(pairs):
                S.op("pe", lambda e, l=l, r=r, i=i: e.matmul(out, lhsT=l, rhs=r, start=(i == 0), stop=(i == n - 1)),
                     reads, writes, sig=(i == n - 1))

        def TR(out, in_, reads, writes, sig):
            S.op("pe", lambda e: e.transpose(out=out, in_=in_, identity=ident[:]), list(reads) + ["ident"], writes, sig=sig)

        def LD(q, out, in_, writes, reads=()):
            S.dma(q, lambda e: e.dma_start(out=out, in_=in_), reads, writes)

        def transpose8(src, ksrc, dst, kdst, evac_eng):
            P, kp = pt.next()
            for k in range(8):
                TR(P[:, k * 128:(k + 1) * 128], src[:, k * 128:(k + 1) * 128], [ksrc], [kp], sig=(k == 7))
            if evac_eng == "act":
                S.op("act", lambda e: e.copy(out=dst, in_=P[:, :].rearrange("p (k t) -> p k t", k=8)), [kp], [kdst])
            else:
                V("dve", "tensor_copy", [kp], [kdst], out=dst, in_=P[:, :].rearrange("p (k t) -> p k t", k=8))

        def rstd_from_ss(ss, kss, n, width):
            ACTV([kss], [kss], out=ss, in_=ss, func=AF.Ln, scale=1.0 / n, bias=EPS)
            ACTV([kss], [kss], out=ss, in_=ss, func=AF.Exp, scale=-0.5)

        for k in range(8):
            LD("pool", Wi3[:, k, :], w_in[k * 128:(k + 1) * 128, :], [("Wi", k)])
        LD("sp", gA[:], g_mix.partition_broadcast(128), ["gA"])
        LD("sp", gB[:], g_ffn.partition_broadcast(128), ["gB"])
        LD("sp", gm_bc[:], g_ml.partition_broadcast(128), ["gm_bc"])
        LD("sp", gh_bc[:], g_hg.partition_broadcast(128), ["gh_bc"])
        LD("sp", bg_bc[:], bg_d.partition_broadcast(128), ["bg_bc"])
        LD("sp", brt_bc[:], brt_d.partition_broadcast(128), ["brt_bc"])
        LD("sp", cw[:], cw_d, ["cw"]); LD("sp", hl[:], hl_d, ["hl"])
        LD("sp", tri[:], c_tri, ["tri"]); LD("sp", blk[:], c_blk, ["blk"]); LD("sp", rmask[:], c_rmask, ["rmask"])
        LD("sp", ec[:], c_ec, ["ec"])
        LD("pool", ident[:], c_ident, ["ident"]); LD("pool", stri[:], c_stri, ["stri"])
        LD("pool", mt[:], c_mt, ["mt"]); LD("sp", ms[:], c_ms, ["ms"])
        LD("pool", Wo3[:, :, :], w_out.rearrange("(k p) n -> p k n", p=128), ["Wo"])
        LD("pool", Wr[:], wrt_d.rearrange("(k p) n -> p k n", p=128), ["Wr"])
        V("pool", "memset", [], ["onesf"], ap=onesf[:], constant=1.0)
        V("pool", "memset", [], ["onesb"], ap=onesb[:], constant=1.0)
        V("pool", "memset", [], ["base"], ap=base[:], constant=0.0)
        V("pool", "memset", [], ["zrow"], ap=zrow[:], constant=0.0)
        S.dma("sp", lambda e: e.dma_start(out=yb_d[ZROW:ZROW + 1, :].rearrange("o (p f) -> (o p) f", p=128), in_=zrow[:]), ["zrow"], ["yb_z"])
        V("dve", "tensor_tensor", ["hl"], ["lbp"], out=lbp[:, :, 0], in0=hl[:, :, 0], in1=hl[:, :, 1], op=ALU.subtract)
        ACTV(["lbp"], ["lbp"], out=lbp[:, :, 0], in_=lbp[:, :, 0], func=AF.Sigmoid)
        V("dve", "tensor_scalar", ["lbp"], ["lbp"], out=lbp[:, :, 1], in0=lbp[:, :, 0], scalar1=-1.0, scalar2=1.0,
          op0=ALU.mult, op1=ALU.add)

        xt_r = AFa.ring("xt", [128, D], 2)
        zqk = AFa.get([128, 8, 131]); tailb = AFa.get([128, 8, 3]); acc = AFa.get([128, 8, 128])
        qs = AFa.get([128, 512]); sgf = AFa.get([128, 512]); lfkk = AFa.get([128, 1024]); lfb = lfkk[:, 0:512]; kk = lfkk[:, 512:1024]
        bb = AFa.get([128, 512]); sig_o_r = [AFa.get([128, 512]) for _ in range(2)]; gs_r = [AFa.get([128, 512]) for _ in range(2)]
        hmog = AFa.get([128, 1024]); og = hmog[:, 512:1024]; ctmp = lfkk.rearrange("p (c t) -> p c t", c=8)
        hm = hmog[:, 0:512].rearrange("p (h e) -> p h e", h=4); Cst = AFa.get([128, 4, 129]); Sf = AFa.get([128, 4, 128])
        sm = AFa.get([128, 128])
        lg = AFa.get([128, 36]); mskt = AFa.get([128, 32]); sel1 = AFa.get([128, 32]); sel2 = AFa.get([128, 32])
        pos = AFa.get([128, 32]); tmp32 = AFa.get([128, 32]); top8 = AFa.get([128, 8])
        hb = ABa.get([128, D]); hT_r = ABa.ring("hT", [128, 8, 128], 2)
        qkT = ABa.get([128, 8, 128]); vext_r = [ABa.get([128, 4, 130]) for _ in range(2)]; hvb_r = [ABa.get([128, 512]) for _ in range(2)]
        qt = ABa.get([128, 512]); kt = ABa.get([128, 512]); kh = ABa.get([128, 512])
        Qblk_r = ABa.ring("Qblk", [128, 4, 128], 4); Vblk_r = [ABa.get([128, 4, 512]) for _ in range(2)]; khT = ABa.get([128, 4, 128])
        ATm = ABa.get([128, 4, 128]); STm = ABa.get([128, 4, 128]); kw = ABa.get([128, 4, 128])
        Cb = ABa.get([128, 4, 130]); Sb = ABa.get([128, 4, 8, 128])
        yb16 = ABa.get([128, D]); yT = ABa.get([128, 8, 128]); h2_r = ABa.ring("h2", [128, D], 2)
        h2T = ABa.get([128, 8, 128]); junk = ABa.get([128, 128]); selb = ABa.get([128, 32])
        ss1 = sm[:, 0:1]; ss2 = sm[:, 1:2]; gt = sm[:, 8:16]; e4 = sm[:, 16:20]; l4 = sm[:, 20:24]
        tmpa = sm[:, 24:32]; aw = sm[:, 32:40]; ebdec = sm[:, 40:48]; ebc = sm[:, 48:52]; dn = sm[:, 52:56]
        sc = sm[:, 56:60]; ssm = sm[:, 60:64]; ssh = sm[:, 64:68]; decs = sm[:, 96:112]
        gmax = sm[:, 68:69]; ngmax = sm[:, 69:70]; ge = sm[:, 72:76]; gsum = sm[:, 76:77]; gval = sm[:, 77:78]
        G4 = sm[:, 80:84]; pen = sm[:, 84:88]; dd = sm[:, 88:89]; e2 = sm[:, 89:90]; w1 = sm[:, 90:91]
        d1f = sm[:, 91:92]; d2f = sm[:, 92:93]
        for q_ in range(2):
            V("pool", "memset", [], [("vext", q_)], ap=vext_r[q_][:, :, 128:130], constant=1.0)
        V("pool", "memset", [], ["Cb"], ap=Cb[:, :, :], constant=0.0)

        import os
        STOP = os.environ.get("K_STOP", "")

        class StopBuild(Exception):
            pass

        def stop_at(tag):
            if STOP == tag:
                raise StopBuild()

        WiK = [("Wi", k) for k in range(8)]
        state = {}

        def A_pre(i):
            xt, kx = xt_r.next()
            LD("sp", xt, x_d[i * 128:(i + 1) * 128, :], [kx])
            ACTV([kx], ["hb", "ss1"], out=hb, in_=xt, func=AF.Square, accum_out=ss1)
            rstd_from_ss(ss1, "ss1", D, 1)
            V("dve", "scalar_tensor_tensor", [kx, "ss1", "gA"], ["hb"], out=hb, in0=xt, scalar=ss1, in1=gA[:],
              op0=ALU.mult, op1=ALU.mult)
            hT, khT = hT_r.next()
            transpose8(hb, "hb", hT, khT, "act")
            state[i] = (xt, kx, hT, khT)

        FMCOL = {"q": 0, "k": 512, "hq": 2056, "hf": 2568}
        TMCOL = {"v": 1024, "o": 1536, "hv": 3080, "hg": 3592}

        def A_grp(i, name):
            xt, kx, hT, khT = state[i]
            q_ = i % 2
            rd = WiK + [khT]
            B, kb = pb.next()
            if name in FMCOL:
                col0 = FMCOL[name]
                for c in range(4):
                    MM(B[:, c * 128:(c + 1) * 128],
                       [(Wi3[:, k, col0 + c * 128:col0 + (c + 1) * 128], hT[:, k, :]) for k in range(8)], rd, [kb])
                B3 = B[:, :].rearrange("p (c t) -> p c t", c=4)
                if name == "q":
                    S.op("act", lambda e, B3=B3: e.copy(out=zqk[:, 0:4, 3:131], in_=B3), [kb], ["zqk"])
                elif name == "k":
                    S.op("act", lambda e, B3=B3: e.copy(out=zqk[:, 4:8, 3:131], in_=B3), [kb], ["zqk"])
                elif name == "hq":
                    S.op("act", lambda e, B=B: e.copy(out=qs, in_=B[:, :]), [kb], ["qs"])
                else:
                    ACTV([kb], ["sgf"], out=sgf, in_=B[:, :], func=AF.Exp, scale=-1.0)
                    V("dve", "tensor_scalar", ["sgf"], ["sgf"], out=sgf, in0=sgf, scalar1=1.0, scalar2=None, op0=ALU.add)
                    V("dve", "reciprocal", ["sgf"], ["sgf"], out=sgf, in_=sgf)
            elif name in TMCOL:
                col0 = TMCOL[name]
                MM(B[:, 0:512], [(hT[:, k, :], Wi3[:, k, col0:col0 + 512]) for k in range(8)], rd, [kb])
                if name == "v":
                    vx = vext_r[q_]
                    S.op("act", lambda e, B=B, vx=vx: e.copy(out=vx[:, :, 0:128], in_=B[:, :].rearrange("p (h e) -> p h e", h=4)),
                         [kb], [("vext", q_)])
                elif name == "o":
                    ACTV([kb], [("sig_o", q_)], out=sig_o_r[q_], in_=B[:, :], func=AF.Exp, scale=-1.0)
                    V("dve", "tensor_scalar", [("sig_o", q_)], [("sig_o", q_)], out=sig_o_r[q_], in0=sig_o_r[q_], scalar1=1.0, scalar2=None, op0=ALU.add)
                    V("dve", "reciprocal", [("sig_o", q_)], [("sig_o", q_)], out=sig_o_r[q_], in_=sig_o_r[q_])
                elif name == "hv":
                    hv_ = hvb_r[q_]; vb_ = Vblk_r[q_]
                    S.op("act", lambda e, B=B, hv_=hv_: e.copy(out=hv_, in_=B[:, :]), [kb], [("hvb", q_)])
                    for c in range(4):
                        ACTV([kb, "ms"], [("Vblk", q_)], out=vb_[:, c, :], in_=B[:, :], func=AF.Copy, scale=ms[:, c:c + 1])
                else:
                    g_ = gs_r[q_]
                    S.op("act", lambda e, B=B, g_=g_: e.copy(out=g_, in_=B[:, :]), [kb], [("gs", q_)])
            else:
                MM(B[:, 0:8], [(hT[:, k, :], Wi3[:, k, 2048:2056]) for k in range(8)], rd, [kb])
                V("dve", "tensor_tensor", [kb, "bg_bc"], ["gt"], out=gt, in0=B[:, 0:8], in1=bg_bc[:], op=ALU.add)

        def conv_silu(i):
            if i % TPS == 0:
                V("pool", "memset", [], ["zqk"], ap=zqk[:, :, 0:3], constant=0.0)
            else:
                V("pool", "tensor_copy", ["tailb"], ["zqk"], out=zqk[:, :, 0:3], in_=tailb)
            for j in range(4):
                wj = cw[:, :, j:j + 1].to_broadcast([128, 8, 128])
                if j == 0:
                    V("dve", "tensor_tensor", ["zqk", "cw"], ["acc"], out=acc, in0=zqk[:, :, 0:128], in1=wj, op=ALU.mult)
                else:
                    V("dve", "tensor_tensor", ["zqk", "cw"], ["lfb", "kk"], out=ctmp, in0=zqk[:, :, j:j + 128], in1=wj, op=ALU.mult)
                    V("dve", "tensor_tensor", ["acc", "lfb", "kk"], ["acc"], out=acc, in0=acc, in1=ctmp, op=ALU.add)
            V("pool", "tensor_copy", ["zqk"], ["tailb"], out=tailb, in_=zqk[:, :, 128:131])

        AORDER = (("v", "o"), ("hv", "hg"), ("q", "k"), ("hq", "hf", "gates"))

        def stageB(i, hook=lambda n: None, part="all"):
            q_ = i % 2
            vext = vext_r[q_]; sig_o = sig_o_r[q_]; hvb = hvb_r[q_]; Vblk = Vblk_r[q_]; gs = gs_r[q_]
            Kv, Kso, Khv, Kvb, Kgs = ("vext", q_), ("sig_o", q_), ("hvb", q_), ("Vblk", q_), ("gs", q_)
            seq_start = (i % TPS == 0)
            par = i % 2
            if part != "main":
                ACTV(["acc"], ["qkT"], out=qkT, in_=acc, func=AF.Silu)
                ACTV(["qs"], ["qs"], out=qs, in_=qs, func=AF.Silu)
                ACTV([Kgs], [Kgs], out=gs, in_=gs, func=AF.Silu)
                V("dve", "tensor_tensor", [Kgs, "gh_bc"], [Kgs], out=gs.rearrange("p (h e) -> p h e", h=4),
                  in0=gs.rearrange("p (h e) -> p h e", h=4),
                  in1=gh_bc[:, :].unsqueeze(1).to_broadcast([128, 4, 128]), op=ALU.mult)
                if seq_start:
                    V("pool", "memset", [], ["Cst"], ap=Cst[:, :, :], constant=0.0)
                    V("pool", "memset", ["Cb"], ["Cb"], ap=Cb[:, :, 0:129], constant=0.0)
                    V("pool", "memset", [], [("Sf", h) for h in range(4)], ap=Sf[:, :, :], constant=0.0)
                    V("pool", "memset", [], [("Sb", h) for h in range(4)], ap=Sb[:, :, (1 - par) * 4 + 3, :], constant=0.0)
                stop_at("B0")
                ACTV(["gt"], ["e4"], out=e4, in_=gt[:, 4:8], func=AF.Exp, scale=-1.0)
                ACTV(["e4"], ["l4"], out=l4, in_=e4, func=AF.Ln, bias=1.0)
                GP, kgp = pb.next()
                MM(GP[:, 0:4], [(tri[:], l4)], ["tri", "l4"], [kgp])
                MM(GP[:, 4:8], [(onesf[:], l4)], ["onesf", "l4"], [kgp])
                V("dve", "tensor_tensor", [kgp, "gt"], ["tmpa"], out=tmpa[:, 0:4], in0=GP[:, 0:4], in1=gt[:, 0:4], op=ALU.add)
                V("dve", "tensor_tensor", [kgp, "tmpa"], ["tmpa"], out=tmpa[:, 4:8], in0=tmpa[:, 0:4], in1=GP[:, 4:8], op=ALU.subtract)
                ACTV(["tmpa"], ["aw"], out=aw, in_=tmpa, func=AF.Exp)
                ACTV([kgp], ["ebdec"], out=ebdec, in_=GP[:, 0:8], func=AF.Exp, scale=-1.0)
                V("dve", "tensor_scalar", ["ebdec"], ["ebc"], out=ebc, in0=ebdec[:, 0:4], scalar1=float(128 ** -0.5), scalar2=None,
                  op0=ALU.mult)
                stop_at("B1")
                for h in range(4):
                    V("dve", "tensor_scalar", ["sgf", "lbp"], ["sgf"], out=sgf[:, h * 128:(h + 1) * 128], in0=sgf[:, h * 128:(h + 1) * 128],
                      scalar1=lbp[:, h, 1:2], scalar2=lbp[:, h, 0:1], op0=ALU.mult, op1=ALU.add)
                ACTV(["sgf"], ["lfb"], out=lfb, in_=sgf, func=AF.Ln)
                V("pool", "tensor_scalar", ["sgf"], ["kk"], out=kk, in0=sgf, scalar1=-1.0, scalar2=1.0, op0=ALU.mult, op1=ALU.add)
                V("dve", "tensor_tensor_scan", ["rmask", "lfb"], ["bb"], out=bb, data0=rmask[:], data1=lfb, initial=0.0,
                  op0=ALU.mult, op1=ALU.add)
                ACTV(["bb", "kk", "lfb"], ["sgf"], out=sgf, in_=bb, func=AF.Exp)
                ACTV(["bb"], ["lfb"], out=lfb, in_=bb, func=AF.Exp, scale=-1.0)
                V("dve", "tensor_tensor", ["qs", "sgf"], ["qt"], out=qt, in0=qs, in1=sgf, op=ALU.mult)
                V("dve", "tensor_tensor", ["kk", "lfb"], ["kt"], out=kt, in0=kk, in1=lfb, op=ALU.mult)
                V("dve", "tensor_tensor", ["kt", "sgf"], ["kh"], out=kh.rearrange("p (g s) -> p g s", s=32),
                  in0=kt.rearrange("p (g s) -> p g s", s=32),
                  in1=sgf.rearrange("p (g s) -> p g s", s=32)[:, :, 31:32].to_broadcast([128, 16, 32]), op=ALU.mult)
                stop_at("B2")
            if part == "pro":
                return
            hook(0)
            H4 = lambda ap: ap.rearrange("p (h t) -> p h t", h=4)
            STb, kst = pb.next()
            for h in range(4):
                MM(STb[:, h * 128:(h + 1) * 128], [(qkT[:, 4 + h, :], qkT[:, h, :])], ["qkT"], [kst])
            KTb, kkt = pt.next()
            for h in range(4):
                TR(KTb[:, h * 128:(h + 1) * 128], qkT[:, 4 + h, :], ["qkT"], [kkt], sig=(h == 3))
            ATb, kat = pb.next()
            for h in range(4):
                MM(ATb[:, h * 128:(h + 1) * 128], [(kt[:, h * 128:(h + 1) * 128], qt[:, h * 128:(h + 1) * 128])], ["kt", "qt"], [kat])
            KHb, kkh = pt.next()
            for h in range(4):
                TR(KHb[:, h * 128:(h + 1) * 128], kh[:, h * 128:(h + 1) * 128], ["kh"], [kkh], sig=(h == 3))
            for h in range(4):
                V("dve", "scalar_tensor_tensor", [kst, "aw", "tri"], [("STm", h)], out=STm[:, h, :], in0=STb[:, h * 128:(h + 1) * 128],
                  scalar=aw[:, h:h + 1], in1=tri[:], op0=ALU.mult, op1=ALU.mult)
            for h in range(4):
                ACTV([kkt, "aw"], [("kw", h)], out=kw[:, h, :], in_=KTb[:, h * 128:(h + 1) * 128], func=AF.Copy, scale=aw[:, 4 + h:5 + h])
            S.op("act", lambda e, KHb=KHb: e.copy(out=khT, in_=H4(KHb[:, 0:512])), [kkh], ["khT"])
            V("dve", "tensor_tensor", [kat, "blk"], ["ATm"], out=ATm, in0=H4(ATb[:, :]),
              in1=blk[:, :].unsqueeze(1).to_broadcast([128, 4, 128]), op=ALU.mult)
            Qbs = []
            for h in range(4):
                Qb, kqb = Qblk_r.next()
                V("dve", "tensor_tensor", ["qt", "mt"], [kqb], out=Qb,
                  in0=qt[:, h * 128:(h + 1) * 128].unsqueeze(1).to_broadcast([128, 4, 128]), in1=mt[:], op=ALU.mult)
                Qbs.append((Qb, kqb))
            V("dve", "tensor_copy", ["sgf"], ["decs"], out=decs, in_=sgf.rearrange("p (g s) -> p g s", s=32)[:, :, 31])
            hook(1)
            PPs = []
            for h in range(4):
                PP, kpp = pb.next()
                for c in range(4):
                    MM(PP[:, c * 128:(c + 1) * 128], [(khT[:, h, :], Vblk[:, c, h * 128:(h + 1) * 128])], ["khT", Kvb], [kpp])
                PPs.append((PP, kpp))
            for c in range(4):
                for h in range(4):
                    PP, kpp = PPs[h]
                    ksf, ksb = ("Sf", h), ("Sb", h)
                    V("dve", "scalar_tensor_tensor", [ksf, "decs", kpp], [ksf], out=Sf[:, h, :], in0=Sf[:, h, :],
                      scalar=decs[:, h * 4 + c:h * 4 + c + 1], in1=PP[:, c * 128:(c + 1) * 128], op0=ALU.mult, op1=ALU.add)
                    S.op("act", lambda e, h=h, c=c: e.copy(out=Sb[:, h, par * 4 + c, :], in_=Sf[:, h, :]), [ksf], [ksb])
            NUs = []
            for hp in range(2):
                NU, knu = pb.next()
                for hh in range(2):
                    h = 2 * hp + hh
                    MM(NU[:, hh * 129:(hh + 1) * 129], [(STm[:, h, :], vext[:, h, 0:129]), (qkT[:, h, :], Cb[:, h, 0:129])],
                       [("STm", h), Kv, "qkT", "Cb"], [knu])
                NUs.append((NU, knu))
            for hp in range(2):
                NU, knu = NUs[hp]
                V("dve", "tensor_tensor", [knu, "ebc"], ["dn"], out=dn[:, 2 * hp:2 * hp + 2],
                  in0=NU[:, 0:258].rearrange("p (h e) -> p h e", h=2)[:, :, 128], in1=ebc[:, 2 * hp:2 * hp + 2], op=ALU.mult)
            V("dve", "tensor_tensor", ["dn"], ["dn"], out=dn, in0=dn, in1=dn, op=ALU.mult)
            V("dve", "tensor_scalar", ["dn"], ["dn"], out=dn, in0=dn, scalar1=1.0, scalar2=None, op0=ALU.max)
            ACTV(["dn"], ["dn"], out=dn, in_=dn, func=AF.Ln)
            ACTV(["dn"], ["dn"], out=dn, in_=dn, func=AF.Exp, scale=-0.5)
            V("dve", "tensor_tensor", ["dn", "ebc"], ["sc"], out=sc, in0=dn, in1=ebc, op=ALU.mult)
            for h in range(4):
                NU, knu = NUs[h // 2]
                o_ = (h % 2) * 129
                V("dve", "scalar_tensor_tensor", [knu, "sc", Kso], ["hm"], out=hm[:, h, :], in0=NU[:, o_:o_ + 128],
                  scalar=sc[:, h:h + 1], in1=sig_o[:, h * 128:(h + 1) * 128], op0=ALU.mult, op1=ALU.mult)
                ACTV(["hm"], ["junk", "ssm"], out=junk[:, 0:128], in_=hm[:, h, :], func=AF.Square, accum_out=ssm[:, h:h + 1])
            hook(2)
            CUs = []
            for hp in range(2):
                CU, kcu = pb.next()
                for hh in range(2):
                    h = 2 * hp + hh
                    MM(CU[:, hh * 129:(hh + 1) * 129], [(kw[:, h, :], vext[:, h, 0:129])], [("kw", h), Kv], [kcu])
                CUs.append((CU, kcu))
            OO, koo = pb.next()
            for h in range(4):
                Qb, kqb = Qbs[h]
                pairs = [(ATm[:, h, :], hvb[:, h * 128:(h + 1) * 128]), (Qb[:, 0, :], Sb[:, h, (1 - par) * 4 + 3, :])]
                pairs += [(Qb[:, c, :], Sb[:, h, par * 4 + c - 1, :]) for c in range(1, 4)]
                MM(OO[:, h * 128:(h + 1) * 128], pairs, ["ATm", Khv, kqb, ("Sb", h)], [koo])
            for hp in range(2):
                CU, kcu = CUs[hp]
                for hh in range(2):
                    h = 2 * hp + hh
                    V("dve", "scalar_tensor_tensor", ["Cst", "ebdec", kcu], ["Cst"], out=Cst[:, h, :], in0=Cst[:, h, :],
                      scalar=ebdec[:, 4 + h:5 + h], in1=CU[:, hh * 129:(hh + 1) * 129], op0=ALU.mult, op1=ALU.add)
            S.op("act", lambda e: e.copy(out=Cb[:, :, 0:129], in_=Cst), ["Cst"], ["Cb"])
            for h in range(4):
                ACTV([koo], ["junk", "ssh"], out=junk[:, 0:128], in_=OO[:, h * 128:(h + 1) * 128], func=AF.Square,
                     accum_out=ssh[:, h:h + 1])
            V("dve", "tensor_tensor", [koo, Kgs], ["og"], out=og, in0=OO[:, :], in1=gs, op=ALU.mult)
            hook(3)
            rstd_from_ss(ssm, "ssm", 128, 4)
            rstd_from_ss(ssh, "ssh", 128, 4)
            for h in range(4):
                V("dve", "scalar_tensor_tensor", ["hm", "ssm", "gm_bc"], ["yb16"], out=yb16[:, h * 128:(h + 1) * 128],
                  in0=hm[:, h, :], scalar=ssm[:, h:h + 1], in1=gm_bc[:, h * 128:(h + 1) * 128], op0=ALU.mult, op1=ALU.mult)
            V("dve", "tensor_tensor", ["og", "ssh"], ["yb16"], out=H4(yb16[:, 512:1024]), in0=H4(og),
              in1=ssh.unsqueeze(2).to_broadcast([128, 4, 128]), op=ALU.mult)

        def stageC(i):
            xt, kx, hT, khT = state.pop(i)
            if dbg:
                yf = AFt[:, NF - 1024:NF]
                V("dve", "tensor_copy", ["yb16"], ["dbgy"], out=yf, in_=yb16)
                S.dma("sp", lambda e: e.dma_start(out=dbg_d["d_y"][i * 128:(i + 1) * 128, :], in_=yf), ["dbgy"], [], semkey=("dma", "dbgy"), final=True)
            transpose8(yb16, "yb16", yT, "yT", "act")
            for half in range(2):
                B, kb = pb.next()
                MM(B[:, 0:512], [(yT[:, k, :], Wo3[:, k, half * 512:(half + 1) * 512]) for k in range(8)], ["yT", "Wo"], [kb])
                V("dve", "tensor_tensor", [kb, kx], [kx], out=xt[:, half * 512:(half + 1) * 512], in0=B[:, 0:512],
                  in1=xt[:, half * 512:(half + 1) * 512], op=ALU.add)
            S.dma("sp", lambda e: e.dma_start(out=x1s[i * 128:(i + 1) * 128, :], in_=xt), [kx], [], semkey=("dma", "st", kx))
            if dbg:
                S.dma("sp", lambda e: e.dma_start(out=dbg_d["d_x1"][i * 128:(i + 1) * 128, :], in_=xt), [kx], [], semkey=("dma", "dbgx", kx), final=True)
            h2, kh2 = h2_r.next()
            ACTV([kx], [kh2, "ss2"], out=h2, in_=xt, func=AF.Square, accum_out=ss2)
            rstd_from_ss(ss2, "ss2", D, 1)
            V("dve", "scalar_tensor_tensor", [kx, "ss2", "gB"], [kh2], out=h2, in0=xt, scalar=ss2, in1=gB[:],
              op0=ALU.mult, op1=ALU.mult)
            transpose8(h2, kh2, h2T, "h2T", "act")
            S.call(lambda: S.pump(10 ** 9))
            ro = (i % 2) * 256
            RL = rbank[:, ro:ro + 64]; krl = ("pb", "r", i % 2)
            RK = rbank[:, ro + 64:ro + 128]; krk = krl
            MM(RL[:, 0:36], [(h2T[:, k, :], Wr[:, k, :]) for k in range(8)], ["h2T", "Wr"], [krl])
            _cap = S.capture()
            _lst = _cap.__enter__()
            V("dve", "tensor_tensor", [krl, "brt_bc"], ["lg"], out=lg, in0=RL[:, 0:36], in1=brt_bc[:], op=ALU.add)
            if dbg:
                S.dma("sp", lambda e: e.dma_start(out=dbg_d["d_lg"][i * 128:(i + 1) * 128, :], in_=lg), ["lg"], [], semkey=("dma", "dbglg"), final=True)
            V("dve", "tensor_reduce", ["lg"], ["gmax"], out=gmax, in_=lg[:, 0:4], axis=AX.X, op=ALU.max)
            V("dve", "tensor_scalar", ["gmax"], ["ngmax"], out=ngmax, in0=gmax, scalar1=-1.0, scalar2=None, op0=ALU.mult)
            ACTV(["lg", "ngmax"], ["ge", "gsum"], out=ge, in_=lg[:, 0:4], func=AF.Exp, bias=ngmax, scale=1.0, accum_out=gsum)
            V("dve", "reciprocal", ["gsum"], ["gval"], out=gval, in_=gsum)
            V("dve", "tensor_scalar", ["lg", "gmax"], ["G4"], out=G4, in0=lg[:, 0:4], scalar1=gmax, scalar2=None, op0=ALU.is_equal)
            V("dve", "tensor_scalar", ["G4"], ["pen"], out=pen, in0=G4, scalar1=1e30, scalar2=-1e30, op0=ALU.mult, op1=ALU.add)
            V("dve", "tensor_tensor", ["lg", "pen"], ["mskt"], out=mskt.rearrange("p (g j) -> p g j", g=4),
              in0=lg[:, 4:36].rearrange("p (g j) -> p g j", g=4), in1=pen.unsqueeze(2).to_broadcast([128, 4, 8]), op=ALU.add)
            V("dve", "max", ["mskt"], ["top8"], out=top8, in_=mskt)
            V("dve", "tensor_scalar", ["mskt", "top8"], ["sel1"], out=sel1, in0=mskt, scalar1=top8[:, 0:1], scalar2=None, op0=ALU.is_equal)
            V("dve", "tensor_scalar", ["mskt", "top8"], ["sel2"], out=sel2, in0=mskt, scalar1=top8[:, 1:2], scalar2=None, op0=ALU.is_equal)
            V("dve", "tensor_tensor", ["sel1", "sel2"], ["selb"], out=selb, in0=sel1, in1=sel2, op=ALU.add)
            V("dve", "tensor_tensor", ["top8"], ["dd"], out=dd, in0=top8[:, 1:2], in1=top8[:, 0:1], op=ALU.subtract)
            ACTV(["dd"], ["e2"], out=e2, in_=dd, func=AF.Exp)
            V("dve", "tensor_scalar", ["e2"], ["w1"], out=w1, in0=e2, scalar1=1.0, scalar2=None, op0=ALU.add)
            V("dve", "reciprocal", ["w1"], ["w1"], out=w1, in_=w1)
            V("dve", "tensor_tensor", ["w1", "gval"], ["cw1"], out=cw1[:, i:i + 1], in0=w1, in1=gval, op=ALU.mult)
            V("dve", "tensor_tensor", ["cw1", "e2"], ["cw2"], out=cw2[:, i:i + 1], in0=cw1[:, i:i + 1], in1=e2, op=ALU.mult)
            MM(RK[:, 0:32], [(stri[:], selb)], ["stri", "selb"], [krk])
            MM(RK[:, 32:64], [(onesb[:], selb)], ["onesb", "selb"], [krk])
            V("dve", "tensor_tensor", [krk, "base"], ["pos"], out=pos, in0=RK[:, 0:32], in1=base[:], op=ALU.add)
            V("dve", "tensor_tensor", [krk, "base"], ["base"], out=base[:], in0=RK[:, 32:64], in1=base[:], op=ALU.add)
            V("dve", "tensor_scalar", ["pos"], ["tmp32"], out=tmp32, in0=pos, scalar1=float(CAP), scalar2=1e9, op0=ALU.is_ge, op1=ALU.mult)
            V("dve", "tensor_tensor", ["pos", "ec"], ["pos"], out=pos, in0=pos, in1=ec[:], op=ALU.add)
            V("dve", "tensor_tensor", ["pos", "tmp32"], ["pos"], out=pos, in0=pos, in1=tmp32, op=ALU.add)
            V("dve", "tensor_scalar", ["pos"], ["pos"], out=pos, in0=pos, scalar1=float(ZROW), scalar2=None, op0=ALU.min)
            V("dve", "tensor_tensor", ["pos", "sel1"], ["sel1"], out=sel1, in0=sel1, in1=pos, op=ALU.mult)
            V("dve", "tensor_tensor", ["pos", "sel2"], ["sel2"], out=sel2, in0=sel2, in1=pos, op=ALU.mult)
            V("dve", "tensor_reduce", ["sel1"], ["d1f"], out=d1f, in_=sel1, axis=AX.X, op=ALU.add)
            V("dve", "tensor_reduce", ["sel2"], ["d2f"], out=d2f, in_=sel2, axis=AX.X, op=ALU.add)
            V("dve", "tensor_copy", ["d1f"], [("d1i", i)], out=d1i[:, i:i + 1], in_=d1f)
            V("dve", "tensor_copy", ["d2f"], [("d2i", i)], out=d2i[:, i:i + 1], in_=d2f)
            if dbg:
                sl = AFt[:, NF - 1028:NF - 1024]
                V("dve", "tensor_copy", ["d1f"], ["dbgs"], out=sl[:, 0:1], in_=d1f)
                V("dve", "tensor_copy", ["d2f"], ["dbgs"], out=sl[:, 1:2], in_=d2f)
                V("dve", "tensor_copy", ["cw1"], ["dbgs"], out=sl[:, 2:3], in_=cw1[:, i:i + 1])
                V("dve", "tensor_copy", ["cw2"], ["dbgs"], out=sl[:, 3:4], in_=cw2[:, i:i + 1])
                S.dma("sp", lambda e: e.dma_start(out=dbg_d["d_slot"][i * 128:(i + 1) * 128, :], in_=sl), ["dbgs"], [], semkey=("dma", "dbgs"), final=True)
            for di, kd in ((d1i, ("d1i", i)), (d2i, ("d2i", i))):
                S.dma("pool", lambda e, di=di, h2=h2: e.indirect_dma_start(
                    out=xs_d, out_offset=bass.IndirectOffsetOnAxis(ap=di[:, i:i + 1], axis=0), in_=h2, in_offset=None),
                    [kh2, kd], [], semkey=("dma", "scat", kh2, kd[0]))
            _cap.__exit__(None, None, None)
            S.call(lambda _lst=_lst: S.deferred.extend(_lst))

        try:
            A_pre(0)
            for grp in AORDER:
                for nm in grp:
                    A_grp(0, nm)
            conv_silu(0)
            stageB(0, part="pro")

            def hook_for(i):
                def hk(n):
                    for nm in AORDER[n]:
                        A_grp(i + 1, nm)
                    if n == 2:
                        conv_silu(i + 1)
                return hk

            def zip2(la, lb):
                na, nb = len(la), len(lb)
                ia = ib = 0
                while ia < na or ib < nb:
                    if ib >= nb or (ia < na and ia * nb <= ib * na):
                        la[ia](); ia += 1
                    else:
                        lb[ib](); ib += 1
            for i in range(NT):
                if i + 1 < NT:
                    A_pre(i + 1)
                    hk = hook_for(i)
                else:
                    hk = lambda n: None
                stageB(i, hk, part="main")
                with S.capture() as lc_:
                    stageC(i)
                lp_ = []
                if i + 1 < NT:
                    with S.capture() as lp_:
                        stageB(i + 1, part="pro")
                zip2(lc_, lp_)
        except StopBuild:
            pass
        if STOP in ("A", "B", "C", "P1"):
            S.pump(10 ** 9)
            S.flush()
            return nc

        S.barrier()
        AFa.reset(); ABa.reset()
        NWE = 8 * 512
        wslots = []
        for s in range(2):
            o = s * 3 * NWE
            wslots.append((Wi[:, o:o + NWE].rearrange("p (k n) -> p k n", k=8),
                           Wi[:, o + NWE:o + 2 * NWE].rearrange("p (k n) -> p k n", k=8),
                           Wi[:, o + 2 * NWE:o + 3 * NWE].rearrange("p (f n) -> p f n", f=4)))
        Wpg = Wi[:, 6 * NWE:6 * NWE + 8 * D].rearrange("p (k n) -> p k n", k=8)
        Wpp = Wo[:, 0:2 * D].rearrange("p (k n) -> p k n", k=2)
        xr_r = ABa.ring("xr", [128, NB, D], 2); xT_r = ABa.ring("xT", [128, 8, CAP], 2); act_r = ABa.ring("actT", [128, 4, CAP], 2)
        yo_r = AFa.ring("yo", [128, D], 2); sgt_r = AFa.ring("sgt", [128, CAP], 2); stg_r = AFa.ring("stg", [128, 4096], 2)

        wsrc = lambda e: (("g", weg[e].rearrange("(p k) n -> p k n", k=8), 8),
                          ("u", weu[e].rearrange("(p k) n -> p k n", k=8), 8),
                          ("d", wed[e].rearrange("(p f) n -> p f n", f=4), 4))
        staged = {}

        def stage_load(e, j):
            nm, src, kk_ = wsrc(e)[j]
            st, kst = stg_r.next()
            st3 = st.rearrange("p (k n) -> p k n", k=kk_)
            LD("sp", st3, src, [kst])
            staged[(e, j)] = (st3, kst, kk_, nm)

        def cast_w(e, j):
            st3, kst, kk_, nm = staged.pop((e, j))
            dst = wslots[e % 2][j]
            kq = ("wexp", e % 2)
            h_ = kk_ // 2
            S.op("act", lambda en, dst=dst, st3=st3, h_=h_: en.copy(out=dst[:, 0:h_, :], in_=st3[:, 0:h_, :]), [kst], [kq + (nm, 0)])
            V("dve", "tensor_copy", [kst], [kq + (nm, 1)], out=dst[:, h_:, :], in_=st3[:, h_:, :])

        def load_expert(e):
            stage_load(e, 0); stage_load(e, 1); cast_w(e, 0); stage_load(e, 2); cast_w(e, 1); cast_w(e, 2)

        load_expert(0)
        stage_load(1, 0); stage_load(1, 1)
        LD("pool", Wpg, wplg.rearrange("(k p) n -> p k n", p=128), ["Wpg"])
        LD("pool", Wpp, wplp.rearrange("(k p) n -> p k n", p=128), ["Wpp"])
        LD("sp", gA[:], g_pl.partition_broadcast(128), ["gA"])
        LD("sp", gB[:], g_fin.partition_broadcast(128), ["gB"])
        def prep_rows(e):
            xr, kxr = xr_r.next()
            LD("pool", xr, xs_d[e * CAP:(e + 1) * CAP, :].rearrange("(b p) d -> p b d", p=128), [kxr])
            xT, kxT = xT_r.next()
            for b in range(NB):
                P, kp = pt.next()
                for k in range(8):
                    TR(P[:, k * 128:(k + 1) * 128], xr[:, b, k:D:8], [kxr], [kp], sig=(k == 7))
                if b % 2 == 0:
                    S.op("act", lambda en, P=P, b=b, xT=xT: en.copy(out=xT[:, :, b * 128:(b + 1) * 128],
                                                                   in_=P[:, :].rearrange("p (k t) -> p k t", k=8)), [kp], [kxT])
                else:
                    V("dve", "tensor_copy", [kp], [kxT], out=xT[:, :, b * 128:(b + 1) * 128],
                      in_=P[:, :].rearrange("p (k t) -> p k t", k=8))
            return xT, kxT

        xt_cur = prep_rows(0)
        for e in range(NE):
            wg, wu, wd = wslots[e % 2]
            kq = ("wexp", e % 2)
            xT, kxT = xt_cur
            if e + 1 < NE:
                cast_w(e + 1, 0); cast_w(e + 1, 1); stage_load(e + 1, 2)
            aT, kaT = act_r.next()
            for f in range(4):
                Gp, kg = pb.next()
                MM(Gp[:, 0:CAP], [(wg[:, k, f:DE:4], xT[:, k, :]) for k in range(8)], [kq + ("g", 0), kq + ("g", 1), kxT], [kg])
                Up, ku = pb.next()
                MM(Up[:, 0:CAP], [(wu[:, k, f:DE:4], xT[:, k, :]) for k in range(8)], [kq + ("u", 0), kq + ("u", 1), kxT], [ku])
                sg, ksg = sgt_r.next()
                ACTV([kg], [ksg], out=sg, in_=Gp[:, 0:CAP], func=AF.Silu)
                V("dve", "tensor_tensor", [ksg, ku], [kaT], out=aT[:, f, :], in0=sg, in1=Up[:, 0:CAP], op=ALU.mult)
            if e + 1 < NE:
                cast_w(e + 1, 2)
            if e + 2 < NE:
                stage_load(e + 2, 0); stage_load(e + 2, 1)
            if e + 1 < NE:
                xt_cur = prep_rows(e + 1)
            for b in range(NB):
                yo, kyo = yo_r.next()
                for half in range(2):
                    Yp, ky = pb.next()
                    MM(Yp[:, 0:512], [(aT[:, f, b * 128:(b + 1) * 128], wd[:, f, half * 512:(half + 1) * 512]) for f in range(4)],
                       [kaT, kq + ("d", 0), kq + ("d", 1)], [ky])
                    if half == 0:
                        S.op("act", lambda en, yo=yo, Yp=Yp: en.copy(out=yo[:, 0:512], in_=Yp[:, 0:512]), [ky], [kyo])
                    else:
                        V("dve", "tensor_copy", [ky], [kyo], out=yo[:, 512:1024], in_=Yp[:, 0:512])
                r0 = e * CAP + b * 128
                S.dma("act", lambda en, yo=yo, r0=r0: en.dma_start(out=yb_d[r0:r0 + 128, :], in_=yo), [kyo], [], semkey=("dma", "st", kyo))

        if STOP == "P2":
            S.flush()
            return nc
        S.barrier()
        AFa.reset(); ABa.reset()
        x1_r = AFa.ring("x1", [128, D], 3); y1_r = AFa.ring("y1", [128, D], 2); y2_r = AFa.ring("y2", [128, D], 2)
        pf_r = AFa.ring("pf", [128, 256], 3); sg3_r = AFa.ring("sg3", [128, D], 1); ot_r = AFa.ring("ot", [128, D], 2)
        sm3_r = AFa.ring("sm3", [128, 8], 3)
        h3_r = ABa.ring("h3", [128, D], 2); h3T_r = ABa.ring("h3T", [128, 8, 128], 2); pbf_r = ABa.ring("pbf", [128, 256], 2)
        pT_r = ABa.ring("pT", [128, 2, 128], 2); junk3 = ABa.get([128, D])
        p3_ld = {}

        def p3_loads(i):
            x1, kx1 = x1_r.next(); pf, kpf = pf_r.next()
            LD("sp", x1, x1s[i * 128:(i + 1) * 128, :], [kx1])
            LD("sp", pf, p_d[i * 128:(i + 1) * 128, :], [kpf])
            p3_ld[i] = (x1, kx1, pf, kpf)

        def p3_front(i):
            y1, ky1 = y1_r.next(); y2, ky2 = y2_r.next()
            sg3, ksg3 = sg3_r.next(); sm3, ksm3 = sm3_r.next(); h3, kh3 = h3_r.next(); pbf, kpbf = pbf_r.next()
            ss3 = sm3[:, 0:1]; ss4 = sm3[:, 1:2]; kss3 = ksm3 + ("a",); kss4 = ksm3 + ("b",)
            if i == 0:
                p3_loads(0)
            if i + 1 < NT:
                p3_loads(i + 1)
            x1, kx1, pf, kpf = p3_ld.pop(i)
            for yy, kyy, di in ((y1, ky1, d1i), (y2, ky2, d2i)):
                S.dma("pool", lambda e, yy=yy, di=di, i=i: e.indirect_dma_start(
                    out=yy, out_offset=None, in_=yb_d, in_offset=bass.IndirectOffsetOnAxis(ap=di[:, i:i + 1], axis=0)),
                    ["yb_z"], [kyy])
            V("dve", "scalar_tensor_tensor", [kx1, ky1], [kx1], out=x1, in0=y1, scalar=cw1[:, i:i + 1], in1=x1, op0=ALU.mult, op1=ALU.add)
            V("dve", "scalar_tensor_tensor", [kx1, ky2], [kx1], out=x1, in0=y2, scalar=cw2[:, i:i + 1], in1=x1, op0=ALU.mult, op1=ALU.add)
            ACTV([kx1], ["junk3", kss3], out=junk3, in_=x1, func=AF.Square, accum_out=ss3)
            rstd_from_ss(ss3, kss3, D, 1)
            V("dve", "scalar_tensor_tensor", [kx1, kss3, "gA"], [kh3], out=h3, in0=x1, scalar=ss3, in1=gA[:], op0=ALU.mult, op1=ALU.mult)
            h3T, kh3T = h3T_r.next()
            transpose8(h3, kh3, h3T, kh3T, "act")
            S.op("act", lambda e, pf=pf, pbf=pbf: e.copy(out=pbf, in_=pf), [kpf], [kpbf])
            P, kp = pt.next()
            for k in range(2):
                TR(P[:, k * 128:(k + 1) * 128], pbf[:, k * 128:(k + 1) * 128], [kpbf], [kp], sig=(k == 1))
            pT, kpT = pT_r.next()
            V("dve", "tensor_copy", [kp], [kpT], out=pT, in_=P[:, 0:256].rearrange("p (k t) -> p k t", k=2))
            return dict(x1=x1, kx1=kx1, sg3=sg3, ksg3=ksg3, sm3=sm3, ksm3=ksm3, h3T=h3T, kh3T=kh3T, pT=pT, kpT=kpT)

        def p3_back(i, c_):
            x1, kx1, sg3, ksg3, sm3, ksm3 = c_['x1'], c_['kx1'], c_['sg3'], c_['ksg3'], c_['sm3'], c_['ksm3']
            h3T, kh3T, pT, kpT = c_['h3T'], c_['kh3T'], c_['pT'], c_['kpT']
            ss4 = sm3[:, 1:2]; kss4 = ksm3 + ('b',)
            for half in range(2):
                Gp, kg = pb.next()
                MM(Gp[:, 0:512], [(h3T[:, k, :], Wpg[:, k, half * 512:(half + 1) * 512]) for k in range(8)], [kh3T, "Wpg"], [kg])
                Pp, kpp = pb.next()
                MM(Pp[:, 0:512], [(pT[:, k, :], Wpp[:, k, half * 512:(half + 1) * 512]) for k in range(2)], [kpT, "Wpp"], [kpp])
                ACTV([kg], [ksg3], out=sg3[:, half * 512:(half + 1) * 512], in_=Gp[:, 0:512], func=AF.Exp, scale=-1.0)
                V("dve", "tensor_scalar", [ksg3], [ksg3], out=sg3[:, half * 512:(half + 1) * 512],
                  in0=sg3[:, half * 512:(half + 1) * 512], scalar1=1.0, scalar2=None, op0=ALU.add)
                V("dve", "reciprocal", [ksg3], [ksg3], out=sg3[:, half * 512:(half + 1) * 512], in_=sg3[:, half * 512:(half + 1) * 512])
                V("dve", "tensor_tensor", [ksg3, kpp], [ksg3], out=sg3[:, half * 512:(half + 1) * 512],
                  in0=sg3[:, half * 512:(half + 1) * 512], in1=Pp[:, 0:512], op=ALU.mult)
            V("dve", "tensor_tensor", [ksg3, kx1], [kx1], out=x1, in0=x1, in1=sg3, op=ALU.add)
            ACTV([kx1], ["junk3", kss4], out=junk3, in_=x1, func=AF.Square, accum_out=ss4)
            rstd_from_ss(ss4, kss4, D, 1)
            ot, kot = ot_r.next()
            V("dve", "scalar_tensor_tensor", [kx1, kss4, "gB"], [kot], out=ot, in0=x1, scalar=ss4, in1=gB[:], op0=ALU.mult, op1=ALU.mult)
            S.dma("sp", lambda e, ot=ot, i=i: e.dma_start(out=out_d[i * 128:(i + 1) * 128, :], in_=ot), [kot], [], semkey=("dma", "st", kot), final=True)

        def zip_emit(la, lb):
            na, nb = len(la), len(lb)
            ia = ib = 0
            while ia < na or ib < nb:
                if ib >= nb or (ia < na and ia * nb <= ib * na):
                    la[ia](); ia += 1
                else:
                    lb[ib](); ib += 1

        ctx3 = p3_front(0)
        for i in range(NT):
            nxt = None
            lf_ = []
            if i + 1 < NT:
                with S.capture() as lf_:
                    nxt = p3_front(i + 1)
            with S.capture() as lb_:
                p3_back(i, ctx3)
            zip_emit(lb_, lf_)
            ctx3 = nxt
        S.flush()
    return nc


def _consts(CAP):
    s = np.arange(128)
    tri = (s[:, None] <= s[None, :]).astype(np.float32)
    stri = (s[:, None] < s[None, :]).astype(np.float32)
    blk = tri * ((s[:, None] // 32) == (s[None, :] // 32))
    rmask = np.tile(((np.arange(512) % 32) != 0).astype(np.float32)[None, :], (128, 1))
    mt = np.zeros((128, 4, 128), np.float32)
    ms = np.zeros((128, 4), np.float32)
    for c in range(4):
        mt[:, c, c * 32:(c + 1) * 32] = 1.0
        ms[c * 32:(c + 1) * 32, c] = 1.0
    ec = np.tile((np.arange(32) * CAP).astype(np.float32)[None, :], (128, 1))
    return {"c_ident": np.eye(128, dtype=np.float32), "c_tri": tri, "c_blk": blk.astype(np.float32), "c_stri": stri,
            "c_rmask": rmask, "c_mt": mt, "c_ms": ms, "c_ec": ec}


def make_in_maps(inp, n_cores, seqs_per_core):
    f = lambda a: np.ascontiguousarray(np.asarray(a, dtype=np.float32))
    x = f(inp["x"]); p = f(inp["p"])[0]
    B, T, _ = x.shape
    shared = {
        "w_in": f(inp["w_in"])[0], "w_out": f(inp["w_out"])[0],
        "wrt": f(np.concatenate([inp["w_rg"][0], inp["w_re"][0]], axis=1)),
        "brt": f(np.concatenate([inp["b_rg"][0], inp["b_re"][0]], axis=0))[None, :],
        "cw": f(np.asarray(inp["conv_qk"])[0].T.reshape(8, 128, 4).transpose(1, 0, 2)),
        "hl": f(np.asarray(inp["hg_lb"]).reshape(2, 4, 128).transpose(2, 1, 0)),
        "g_mix": f(inp["g_mix"])[0:1], "g_ffn": f(inp["g_ffn"])[0:1], "g_pl": f(inp["g_pl"])[0:1],
        "g_final": f(inp["g_final"])[None, :], "g_mlstm": f(inp["g_mlstm"])[0:1], "g_hgrn": f(inp["g_hgrn"])[0:1],
        "b_mgate": f(inp["b_mgate"])[0:1],
        "w_e_gate": f(inp["w_e_gate"])[0], "w_e_up": f(inp["w_e_up"])[0], "w_e_down": f(inp["w_e_down"])[0],
        "w_pl_gate": f(inp["w_pl_gate"])[0], "w_pl_proj": f(inp["w_pl_proj"])[0],
    }
    maps = []
    for c in range(n_cores):
        b0 = c * seqs_per_core
        m = dict(shared)
        m["x"] = f(x[b0:b0 + seqs_per_core].reshape(seqs_per_core * T, D))
        m["p"] = f(p[b0:b0 + seqs_per_core].reshape(seqs_per_core * T, 256))
        maps.append(m)
    return maps


def run(inp, n_cores, seqs_per_core, CAP, dbg=False):
    x = np.asarray(inp["x"])
    B, T, _ = x.shape
    TPS = T // 128
    NT = TPS * seqs_per_core
    nc = build_nc(NT, TPS, CAP, dbg=dbg)
    maps = make_in_maps(inp, n_cores, seqs_per_core)
    cst = _consts(CAP)
    for m in maps:
        m.update(cst)
    res = run_bass_kernel_spmd(nc, maps, core_ids=list(range(n_cores)))
    out = np.concatenate([np.asarray(r["out"]).reshape(seqs_per_core, T, D) for r in res.results], axis=0)
    if dbg:
        return out.astype(np.float32), res.results
    return out.astype(np.float32)


def kernel(**inputs):
    return run(inputs, 8, 2, 512)
```

```python
import numpy as np
from contextlib import ExitStack
import concourse.bass as bass
import concourse.mybir as mybir
from concourse.bass_utils import run_bass_kernel_spmd

F32 = mybir.dt.float32
BF16 = mybir.dt.bfloat16
I32 = mybir.dt.int32
AF = mybir.ActivationFunctionType
ALU = mybir.AluOpType
AX = mybir.AxisListType

ENGS = ("pe", "act", "dve", "pool", "sp")
EPS = 1e-6
D = 1024
NE = 32
DE = 512


class Sched:
    def __init__(self, nc, es, n_dma_sems=80, strict_same=True):
        self.nc = nc
        self.ops = {e: [] for e in ENGS}
        self.cnt = {}
        self.sem = {}
        self.seen = {}
        self.res = {}
        self.strict_same = strict_same
        for e in ENGS:
            self.sem[e] = es.enter_context(nc.semaphore("s_" + e))
            self.cnt[e] = 0
        self.free_sems = [es.enter_context(nc.semaphore("d%d" % i)) for i in range(n_dma_sems)]
        self.final_waits = []
        self.cap = None
        self.deferred = []
        self.pumping = False

    def _r(self, key):
        r = self.res.get(key)
        if r is None:
            r = self.res[key] = {"w": None, "r": {}}
        return r

    def _deps(self, reads, writes):
        deps = []
        for k in reads:
            r = self._r(k)
            if r["w"] is not None:
                deps.append(r["w"])
            if isinstance(k, tuple) and k[0] in ("pb", "pt"):
                deps.extend(r["r"].items())
        for k in writes:
            r = self._r(k)
            if r["w"] is not None:
                deps.append(r["w"])
            deps.extend(r["r"].items())
        return deps

    def _waits(self, eng, deps):
        best = {}
        for (x, v) in deps:
            if x == eng and (not self.strict_same or eng == "pe"):
                continue
            if v > best.get(x, 0):
                best[x] = v
        out = []
        for x, v in best.items():
            if v > self.seen.get((eng, x), 0):
                self.seen[(eng, x)] = v
                out.append((self.sem[x], v))
        return out

    def _commit(self, stamp, reads, writes):
        x, v = stamp
        for k in reads:
            r = self._r(k)
            if r["r"].get(x, 0) < v:
                r["r"][x] = v
        for k in writes:
            r = self._r(k)
            r["w"] = (x, v)
            r["r"] = {}

    def op(self, eng, fn, reads=(), writes=(), sig=True):
        if self.cap is not None:
            self.cap.append(lambda: self.op(eng, fn, reads, writes, sig))
            return
        self._op(eng, fn, reads, writes, sig)
        if eng == "dve" and self.deferred and not self.pumping:
            self.pump(1)

    def pump(self, n):
        self.pumping = True
        while n > 0 and self.deferred:
            self.deferred.pop(0)()
            n -= 1
        self.pumping = False

    def capture(self):
        sch = self

        class _C:
            def __enter__(s_):
                s_.prev = sch.cap
                sch.cap = []
                s_.lst = sch.cap
                return s_.lst

            def __exit__(s_, *a):
                sch.cap = s_.prev
                return False
        return _C()

    def call(self, fn):
        if self.cap is not None:
            self.cap.append(fn)
        else:
            fn()

    def _op(self, eng, fn, reads=(), writes=(), sig=True):
        waits = self._waits(eng, self._deps(reads, writes))
        if sig:
            self.cnt[eng] += 1
            stamp = (eng, self.cnt[eng])
            inc = (self.sem[eng], 1)
        else:
            stamp = (eng, self.cnt[eng] + 1)
            inc = None
        self.ops[eng].append((waits, fn, inc))
        self._commit(stamp, reads, writes)

    def dma(self, q, fn, reads=(), writes=(), semkey=None, final=False):
        if self.cap is not None:
            self.cap.append(lambda: self.dma(q, fn, reads, writes, semkey, final))
            return
        if semkey is None:
            semkey = ("dma", (tuple(writes) + tuple(reads))[0])
        if semkey not in self.sem:
            self.sem[semkey] = self.free_sems.pop()
            self.cnt[semkey] = 0
        waits = self._waits(q, self._deps(reads, writes))
        self.cnt[semkey] += 16
        stamp = (semkey, self.cnt[semkey])
        self.ops[q].append((waits, fn, (self.sem[semkey], 16)))
        self._commit(stamp, reads, writes)
        if final:
            self.final_waits.append(stamp)

    def barrier(self):
        self.pump(10 ** 9)
        tot = [(x, v) for x, v in self.cnt.items() if v > 0]
        for e in ENGS:
            waits = self._waits(e, tot)
            if waits:
                self.ops[e].append((waits, None, None))
        self.res = {}

    def flush(self):
        nc = self.nc
        fin = [(self.sem[x], v) for x, v in self.cnt.items() if v > 0 and x != "sp"]
        ops = self.ops

        def run(engine, lst, tail=()):
            for (waits, fn, inc) in lst:
                for (s, v) in waits:
                    engine.wait_ge(s, v)
                if fn is None:
                    continue
                ins = fn(engine)
                if inc is not None:
                    ins.then_inc(inc[0], inc[1])
            for (s, v) in tail:
                engine.wait_ge(s, v)

        with nc.Block() as block:
            @block.sync
            def _(e):
                run(e, ops["sp"], fin)

            @block.scalar
            def _(e):
                run(e, ops["act"])

            @block.vector
            def _(e):
                run(e, ops["dve"])

            @block.gpsimd
            def _(e):
                run(e, ops["pool"])

            @block.tensor
            def _(e):
                run(e, ops["pe"])


class Ring:
    sched = None

    def __init__(self, name, aps):
        self.name, self.t, self.i = name, aps, -1

    def next(self):
        self.i = (self.i + 1) % len(self.t)
        key = (self.name, self.i)
        S_ = Ring.sched
        if S_ is not None and S_.cap is None and self.name in ("pb", "pt"):
            r = S_.res.get(key)
            assert r is None or r["w"] is None or len(r["r"]) > 0, ("PSUM ring slot reused before its content was read", key)
        return self.t[self.i], key


class Arena:
    def __init__(self, t, n):
        self.t, self.n, self.off = t, n, 0

    def reset(self):
        self.off = 0

    def get(self, shape):
        n = int(np.prod(shape[1:]))
        n_al = (n + 15) // 16 * 16
        assert self.off + n_al <= self.n, ("arena overflow", self.off, n_al, self.n)
        ap = self.t[:, self.off:self.off + n]
        self.off += n_al
        if len(shape) == 3:
            ap = ap.rearrange("p (a b) -> p a b", a=shape[1])
        elif len(shape) == 4:
            ap = ap.rearrange("p (a b c) -> p a b c", a=shape[1], b=shape[2])
        return ap

    def ring(self, name, shape, n):
        return Ring(name, [self.get(shape) for _ in range(n)])


def build_nc(NT, TPS, CAP, dbg=False):
    NTOK = NT * 128
    NB = CAP // 128
    ZROW = NE * CAP
    nc = bass.Bass("TRN2", target_bir_lowering=False)

    def din(name, shape, dt=F32):
        return nc.dram_tensor(name, list(shape), dt, kind="ExternalInput").ap()

    x_d = din("x", [NTOK, D]); p_d = din("p", [NTOK, 256])
    w_in = din("w_in", [D, 4104]); w_out = din("w_out", [D, D])
    wrt_d = din("wrt", [D, 36]); brt_d = din("brt", [1, 36])
    cw_d = din("cw", [128, 8, 4]); hl_d = din("hl", [128, 4, 2])
    g_mix = din("g_mix", [1, D]); g_ffn = din("g_ffn", [1, D]); g_pl = din("g_pl", [1, D]); g_fin = din("g_final", [1, D])
    g_ml = din("g_mlstm", [1, 512]); g_hg = din("g_hgrn", [1, 128]); bg_d = din("b_mgate", [1, 8])
    weg = din("w_e_gate", [NE, D, DE]); weu = din("w_e_up", [NE, D, DE]); wed = din("w_e_down", [NE, DE, D])
    wplg = din("w_pl_gate", [D, D]); wplp = din("w_pl_proj", [256, D])
    c_ident = din("c_ident", [128, 128]); c_tri = din("c_tri", [128, 128]); c_blk = din("c_blk", [128, 128])
    c_stri = din("c_stri", [128, 128]); c_rmask = din("c_rmask", [128, 512])
    c_mt = din("c_mt", [128, 4, 128]); c_ms = din("c_ms", [128, 4]); c_ec = din("c_ec", [128, 32])
    out_d = nc.dram_tensor("out", [NTOK, D], F32, kind="ExternalOutput").ap()
    x1s = nc.dram_tensor("x1s", [NTOK, D], F32, kind="Internal").ap()
    xs_d = nc.dram_tensor("xs", [ZROW + 1, D], BF16, kind="Internal").ap()
    yb_d = nc.dram_tensor("yb", [ZROW + 1, D], F32, kind="Internal").ap()
    dbg_d = {}
    if dbg:
        for nm, shp in (("d_y", [NTOK, D]), ("d_x1", [NTOK, D]), ("d_lg", [NTOK, 36]), ("d_slot", [NTOK, 4])):
            dbg_d[nm] = nc.dram_tensor(nm, shp, F32, kind="ExternalOutput").ap()

    with ExitStack() as es:
        S = Sched(nc, es)
        Ring.sched = S
        sbt = lambda name, shape, dt: es.enter_context(nc.sbuf_tensor("sb_" + name, shape, dt))
        Wi = sbt("Wi", [128, 8 * 4104], BF16)
        Wi3 = Wi[:, :].rearrange("p (k n) -> p k n", k=8)
        Wo = sbt("Wo", [128, 8 * D], BF16)
        Wo3 = Wo[:, :].rearrange("p (k n) -> p k n", k=8)
        Wr = sbt("Wr", [128, 8, 36], BF16)
        gA = sbt("gA", [128, D], F32)
        gB = sbt("gB", [128, D], F32)
        gm_bc = sbt("gm_bc", [128, 512], F32)
        gh_bc = sbt("gh_bc", [128, 128], F32)
        bg_bc = sbt("bg_bc", [128, 8], F32)
        brt_bc = sbt("brt_bc", [128, 36], F32)
        cw = sbt("cw", [128, 8, 4], F32)
        hl = sbt("hl", [128, 4, 2], F32)
        lbp = sbt("lbp", [128, 4, 2], F32)
        ident = sbt("ident", [128, 128], BF16)
        tri = sbt("tri", [128, 128], F32)
        onesf = sbt("onesf", [128, 128], F32)
        blk = sbt("blk", [128, 128], F32)
        stri = sbt("stri", [128, 128], BF16)
        onesb = sbt("onesb", [128, 128], BF16)
        rmask = sbt("rmask", [128, 512], F32)
        mt = sbt("mt", [128, 4, 128], BF16)
        ms = sbt("ms", [128, 4], F32)
        ec = sbt("ec", [128, 32], F32)
        d1i = sbt("d1i", [128, NT], I32); d2i = sbt("d2i", [128, NT], I32)
        cw1 = sbt("cw1", [128, NT], F32); cw2 = sbt("cw2", [128, NT], F32)
        base = sbt("base", [128, 32], F32)
        zrow = sbt("zrow", [128, 8], F32)
        NF, NBF = (12300 if dbg else 11300), 25900
        AFt = sbt("arenaF", [128, NF], F32); ABt = sbt("arenaB", [128, NBF], BF16)
        AFa, ABa = Arena(AFt, NF), Arena(ABt, NBF)
        pb = Ring("pb", [es.enter_context(nc.psum_tensor("pb%d" % i, [128, 512], F32)) for i in range(5)])
        rbank = es.enter_context(nc.psum_tensor("rbank", [128, 512], F32))
        pt = Ring("pt", [es.enter_context(nc.psum_tensor("pt%d" % i, [128, 1024], BF16)) for i in range(2)])

        def V(eng, name, reads, writes, **kw):
            S.op(eng, lambda e: getattr(e, name)(**kw), reads, writes)

        def ACTV(reads, writes, **kw):
            S.op("act", lambda e: e.activation(**kw), reads, writes)

        def MM(out, pairs, reads, writes):
            n = len(pairs)
            for i, (l, r) in enumerate(pairs):
                S.op("pe", lambda e, l=l, r=r, i=i: e.matmul(out, lhsT=l, rhs=r, start=(i == 0), stop=(i == n - 1)),
                     reads, writes, sig=(i == n - 1))

        def TR(out, in_, reads, writes, sig):
            S.op("pe", lambda e: e.transpose(out=out, in_=in_, identity=ident[:]), list(reads) + ["ident"], writes, sig=sig)

        def LD(q, out, in_, writes, reads=()):
            S.dma(q, lambda e: e.dma_start(out=out, in_=in_), reads, writes)

        def transpose8(src, ksrc, dst, kdst, evac_eng):
            P, kp = pt.next()
            for k in range(8):
                TR(P[:, k * 128:(k + 1) * 128], src[:, k * 128:(k + 1) * 128], [ksrc], [kp], sig=(k == 7))
            if evac_eng == "act":
                S.op("act", lambda e: e.copy(out=dst, in_=P[:, :].rearrange("p (k t) -> p k t", k=8)), [kp], [kdst])
            else:
                V("dve", "tensor_copy", [kp], [kdst], out=dst, in_=P[:, :].rearrange("p (k t) -> p k t", k=8))

        def rstd_from_ss(ss, kss, n, width):
            ACTV([kss], [kss], out=ss, in_=ss, func=AF.Ln, scale=1.0 / n, bias=EPS)
            ACTV([kss], [kss], out=ss, in_=ss, func=AF.Exp, scale=-0.5)

        for k in range(8):
            LD("pool", Wi3[:, k, :], w_in[k * 128:(k + 1) * 128, :], [("Wi", k)])
        LD("sp", gA[:], g_mix.partition_broadcast(128), ["gA"])
        LD("sp", gB[:], g_ffn.partition_broadcast(128), ["gB"])
        LD("sp", gm_bc[:], g_ml.partition_broadcast(128), ["gm_bc"])
        LD("sp", gh_bc[:], g_hg.partition_broadcast(128), ["gh_bc"])
        LD("sp", bg_bc[:], bg_d.partition_broadcast(128), ["bg_bc"])
        LD("sp", brt_bc[:], brt_d.partition_broadcast(128), ["brt_bc"])
        LD("sp", cw[:], cw_d, ["cw"]); LD("sp", hl[:], hl_d, ["hl"])
        LD("sp", tri[:], c_tri, ["tri"]); LD("sp", blk[:], c_blk, ["blk"]); LD("sp", rmask[:], c_rmask, ["rmask"])
        LD("sp", ec[:], c_ec, ["ec"])
        LD("pool", ident[:], c_ident, ["ident"]); LD("pool", stri[:], c_stri, ["stri"])
        LD("pool", mt[:], c_mt, ["mt"]); LD("sp", ms[:], c_ms, ["ms"])
        LD("pool", Wo3[:, :, :], w_out.rearrange("(k p) n -> p k n", p=128), ["Wo"])
        LD("pool", Wr[:], wrt_d.rearrange("(k p) n -> p k n", p=128), ["Wr"])
        V("pool", "memset", [], ["onesf"], ap=onesf[:], constant=1.0)
        V("pool", "memset", [], ["onesb"], ap=onesb[:], constant=1.0)
        V("pool", "memset", [], ["base"], ap=base[:], constant=0.0)
        V("pool", "memset", [], ["zrow"], ap=zrow[:], constant=0.0)
        S.dma("sp", lambda e: e.dma_start(out=yb_d[ZROW:ZROW + 1, :].rearrange("o (p f) -> (o p) f", p=128), in_=zrow[:]), ["zrow"], ["yb_z"])
        V("dve", "tensor_tensor", ["hl"], ["lbp"], out=lbp[:, :, 0], in0=hl[:, :, 0], in1=hl[:, :, 1], op=ALU.subtract)
        ACTV(["lbp"], ["lbp"], out=lbp[:, :, 0], in_=lbp[:, :, 0], func=AF.Sigmoid)
        V("dve", "tensor_scalar", ["lbp"], ["lbp"], out=lbp[:, :, 1], in0=lbp[:, :, 0], scalar1=-1.0, scalar2=1.0,
          op0=ALU.mult, op1=ALU.add)

        xt_r = AFa.ring("xt", [128, D], 2)
        zqk = AFa.get([128, 8, 131]); tailb = AFa.get([128, 8, 3]); acc = AFa.get([128, 8, 128])
        qs = AFa.get([128, 512]); sgf = AFa.get([128, 512]); lfkk = AFa.get([128, 1024]); lfb = lfkk[:, 0:512]; kk = lfkk[:, 512:1024]
        bb = AFa.get([128, 512]); sig_o_r = [AFa.get([128, 512]) for _ in range(2)]; gs_r = [AFa.get([128, 512]) for _ in range(2)]
        hmog = AFa.get([128, 1024]); og = hmog[:, 512:1024]; ctmp = lfkk.rearrange("p (c t) -> p c t", c=8)
        hm = hmog[:, 0:512].rearrange("p (h e) -> p h e", h=4); Cst = AFa.get([128, 4, 129]); Sf = AFa.get([128, 4, 128])
        sm = AFa.get([128, 128])
        lg = AFa.get([128, 36]); mskt = AFa.get([128, 32]); sel1 = AFa.get([128, 32]); sel2 = AFa.get([128, 32])
        pos = AFa.get([128, 32]); tmp32 = AFa.get([128, 32]); top8 = AFa.get([128, 8])
        hb = ABa.get([128, D]); hT_r = ABa.ring("hT", [128, 8, 128], 2)
        qkT = ABa.get([128, 8, 128]); vext_r = [ABa.get([128, 4, 130]) for _ in range(2)]; hvb_r = [ABa.get([128, 512]) for _ in range(2)]
        qt = ABa.get([128, 512]); kt = ABa.get([128, 512]); kh = ABa.get([128, 512])
        Qblk_r = ABa.ring("Qblk", [128, 4, 128], 4); Vblk_r = [ABa.get([128, 4, 512]) for _ in range(2)]; khT = ABa.get([128, 4, 128])
        ATm = ABa.get([128, 4, 128]); STm = ABa.get([128, 4, 128]); kw = ABa.get([128, 4, 128])
        Cb = ABa.get([128, 4, 130]); Sb = ABa.get([128, 4, 8, 128])
        yb16 = ABa.get([128, D]); yT = ABa.get([128, 8, 128]); h2_r = ABa.ring("h2", [128, D], 2)
        h2T = ABa.get([128, 8, 128]); junk = ABa.get([128, 128]); selb = ABa.get([128, 32])
        ss1 = sm[:, 0:1]; ss2 = sm[:, 1:2]; gt = sm[:, 8:16]; e4 = sm[:, 16:20]; l4 = sm[:, 20:24]
        tmpa = sm[:, 24:32]; aw = sm[:, 32:40]; ebdec = sm[:, 40:48]; ebc = sm[:, 48:52]; dn = sm[:, 52:56]
        sc = sm[:, 56:60]; ssm = sm[:, 60:64]; ssh = sm[:, 64:68]; decs = sm[:, 96:112]
        gmax = sm[:, 68:69]; ngmax = sm[:, 69:70]; ge = sm[:, 72:76]; gsum = sm[:, 76:77]; gval = sm[:, 77:78]
        G4 = sm[:, 80:84]; pen = sm[:, 84:88]; dd = sm[:, 88:89]; e2 = sm[:, 89:90]; w1 = sm[:, 90:91]
        d1f = sm[:, 91:92]; d2f = sm[:, 92:93]
        for q_ in range(2):
            V("pool", "memset", [], [("vext", q_)], ap=vext_r[q_][:, :, 128:130], constant=1.0)
        V("pool", "memset", [], ["Cb"], ap=Cb[:, :, :], constant=0.0)

        import os
        STOP = os.environ.get("K_STOP", "")

        class StopBuild(Exception):
            pass

        def stop_at(tag):
            if STOP == tag:
                raise StopBuild()

        WiK = [("Wi", k) for k in range(8)]
        state = {}

        def A_pre(i):
            xt, kx = xt_r.next()
            LD("sp", xt, x_d[i * 128:(i + 1) * 128, :], [kx])
            ACTV([kx], ["hb", "ss1"], out=hb, in_=xt, func=AF.Square, accum_out=ss1)
            rstd_from_ss(ss1, "ss1", D, 1)
            V("dve", "scalar_tensor_tensor", [kx, "ss1", "gA"], ["hb"], out=hb, in0=xt, scalar=ss1, in1=gA[:],
              op0=ALU.mult, op1=ALU.mult)
            hT, khT = hT_r.next()
            transpose8(hb, "hb", hT, khT, "act")
            state[i] = (xt, kx, hT, khT)

        FMCOL = {"q": 0, "k": 512, "hq": 2056, "hf": 2568}
        TMCOL = {"v": 1024, "o": 1536, "hv": 3080, "hg": 3592}

        def A_grp(i, name):
            xt, kx, hT, khT = state[i]
            q_ = i % 2
            rd = WiK + [khT]
            B, kb = pb.next()
            if name in FMCOL:
                col0 = FMCOL[name]
                for c in range(4):
                    MM(B[:, c * 128:(c + 1) * 128],
                       [(Wi3[:, k, col0 + c * 128:col0 + (c + 1) * 128], hT[:, k, :]) for k in range(8)], rd, [kb])
                B3 = B[:, :].rearrange("p (c t) -> p c t", c=4)
                if name == "q":
                    S.op("act", lambda e, B3=B3: e.copy(out=zqk[:, 0:4, 3:131], in_=B3), [kb], ["zqk"])
                elif name == "k":
                    S.op("act", lambda e, B3=B3: e.copy(out=zqk[:, 4:8, 3:131], in_=B3), [kb], ["zqk"])
                elif name == "hq":
                    S.op("act", lambda e, B=B: e.copy(out=qs, in_=B[:, :]), [kb], ["qs"])
                else:
                    ACTV([kb], ["sgf"], out=sgf, in_=B[:, :], func=AF.Exp, scale=-1.0)
                    V("dve", "tensor_scalar", ["sgf"], ["sgf"], out=sgf, in0=sgf, scalar1=1.0, scalar2=None, op0=ALU.add)
                    V("dve", "reciprocal", ["sgf"], ["sgf"], out=sgf, in_=sgf)
            elif name in TMCOL:
                col0 = TMCOL[name]
                MM(B[:, 0:512], [(hT[:, k, :], Wi3[:, k, col0:col0 + 512]) for k in range(8)], rd, [kb])
                if name == "v":
                    vx = vext_r[q_]
                    S.op("act", lambda e, B=B, vx=vx: e.copy(out=vx[:, :, 0:128], in_=B[:, :].rearrange("p (h e) -> p h e", h=4)),
                         [kb], [("vext", q_)])
                elif name == "o":
                    ACTV([kb], [("sig_o", q_)], out=sig_o_r[q_], in_=B[:, :], func=AF.Exp, scale=-1.0)
                    V("dve", "tensor_scalar", [("sig_o", q_)], [("sig_o", q_)], out=sig_o_r[q_], in0=sig_o_r[q_], scalar1=1.0, scalar2=None, op0=ALU.add)
                    V("dve", "reciprocal", [("sig_o", q_)], [("sig_o", q_)], out=sig_o_r[q_], in_=sig_o_r[q_])
                elif name == "hv":
                    hv_ = hvb_r[q_]; vb_ = Vblk_r[q_]
                    S.op("act", lambda e, B=B, hv_=hv_: e.copy(out=hv_, in_=B[:, :]), [kb], [("hvb", q_)])
                    for c in range(4):
                        ACTV([kb, "ms"], [("Vblk", q_)], out=vb_[:, c, :], in_=B[:, :], func=AF.Copy, scale=ms[:, c:c + 1])
                else:
                    g_ = gs_r[q_]
                    S.op("act", lambda e, B=B, g_=g_: e.copy(out=g_, in_=B[:, :]), [kb], [("gs", q_)])
            else:
                MM(B[:, 0:8], [(hT[:, k, :], Wi3[:, k, 2048:2056]) for k in range(8)], rd, [kb])
                V("dve", "tensor_tensor", [kb, "bg_bc"], ["gt"], out=gt, in0=B[:, 0:8], in1=bg_bc[:], op=ALU.add)

        def conv_silu(i):
            if i % TPS == 0:
                V("pool", "memset", [], ["zqk"], ap=zqk[:, :, 0:3], constant=0.0)
            else:
                V("pool", "tensor_copy", ["tailb"], ["zqk"], out=zqk[:, :, 0:3], in_=tailb)
            for j in range(4):
                wj = cw[:, :, j:j + 1].to_broadcast([128, 8, 128])
                if j == 0:
                    V("dve", "tensor_tensor", ["zqk", "cw"], ["acc"], out=acc, in0=zqk[:, :, 0:128], in1=wj, op=ALU.mult)
                else:
                    V("dve", "tensor_tensor", ["zqk", "cw"], ["lfb", "kk"], out=ctmp, in0=zqk[:, :, j:j + 128], in1=wj, op=ALU.mult)
                    V("dve", "tensor_tensor", ["acc", "lfb", "kk"], ["acc"], out=acc, in0=acc, in1=ctmp, op=ALU.add)
            V("pool", "tensor_copy", ["zqk"], ["tailb"], out=tailb, in_=zqk[:, :, 128:131])

        AORDER = (("v", "o"), ("hv", "hg"), ("q", "k"), ("hq", "hf", "gates"))

        def stageB(i, hook=lambda n: None, part="all"):
            q_ = i % 2
            vext = vext_r[q_]; sig_o = sig_o_r[q_]; hvb = hvb_r[q_]; Vblk = Vblk_r[q_]; gs = gs_r[q_]
            Kv, Kso, Khv, Kvb, Kgs = ("vext", q_), ("sig_o", q_), ("hvb", q_), ("Vblk", q_), ("gs", q_)
            seq_start = (i % TPS == 0)
            par = i % 2
            if part != "main":
                ACTV(["acc"], ["qkT"], out=qkT, in_=acc, func=AF.Silu)
                ACTV(["qs"], ["qs"], out=qs, in_=qs, func=AF.Silu)
                ACTV([Kgs], [Kgs], out=gs, in_=gs, func=AF.Silu)
                V("dve", "tensor_tensor", [Kgs, "gh_bc"], [Kgs], out=gs.rearrange("p (h e) -> p h e", h=4),
                  in0=gs.rearrange("p (h e) -> p h e", h=4),
                  in1=gh_bc[:, :].unsqueeze(1).to_broadcast([128, 4, 128]), op=ALU.mult)
                if seq_start:
                    V("pool", "memset", [], ["Cst"], ap=Cst[:, :, :], constant=0.0)
                    V("pool", "memset", ["Cb"], ["Cb"], ap=Cb[:, :, 0:129], constant=0.0)
                    V("pool", "memset", [], [("Sf", h) for h in range(4)], ap=Sf[:, :, :], constant=0.0)
                    V("pool", "memset", [], [("Sb", h) for h in range(4)], ap=Sb[:, :, (1 - par) * 4 + 3, :], constant=0.0)
                stop_at("B0")
                ACTV(["gt"], ["e4"], out=e4, in_=gt[:, 4:8], func=AF.Exp, scale=-1.0)
                ACTV(["e4"], ["l4"], out=l4, in_=e4, func=AF.Ln, bias=1.0)
                GP, kgp = pb.next()
                MM(GP[:, 0:4], [(tri[:], l4)], ["tri", "l4"], [kgp])
                MM(GP[:, 4:8], [(onesf[:], l4)], ["onesf", "l4"], [kgp])
                V("dve", "tensor_tensor", [kgp, "gt"], ["tmpa"], out=tmpa[:, 0:4], in0=GP[:, 0:4], in1=gt[:, 0:4], op=ALU.add)
                V("dve", "tensor_tensor", [kgp, "tmpa"], ["tmpa"], out=tmpa[:, 4:8], in0=tmpa[:, 0:4], in1=GP[:, 4:8], op=ALU.subtract)
                ACTV(["tmpa"], ["aw"], out=aw, in_=tmpa, func=AF.Exp)
                ACTV([kgp], ["ebdec"], out=ebdec, in_=GP[:, 0:8], func=AF.Exp, scale=-1.0)
                V("dve", "tensor_scalar", ["ebdec"], ["ebc"], out=ebc, in0=ebdec[:, 0:4], scalar1=float(128 ** -0.5), scalar2=None,
                  op0=ALU.mult)
                stop_at("B1")
                for h in range(4):
                    V("dve", "tensor_scalar", ["sgf", "lbp"], ["sgf"], out=sgf[:, h * 128:(h + 1) * 128], in0=sgf[:, h * 128:(h + 1) * 128],
                      scalar1=lbp[:, h, 1:2], scalar2=lbp[:, h, 0:1], op0=ALU.mult, op1=ALU.add)
                ACTV(["sgf"], ["lfb"], out=lfb, in_=sgf, func=AF.Ln)
                V("pool", "tensor_scalar", ["sgf"], ["kk"], out=kk, in0=sgf, scalar1=-1.0, scalar2=1.0, op0=ALU.mult, op1=ALU.add)
                V("dve", "tensor_tensor_scan", ["rmask", "lfb"], ["bb"], out=bb, data0=rmask[:], data1=lfb, initial=0.0,
                  op0=ALU.mult, op1=ALU.add)
                ACTV(["bb", "kk", "lfb"], ["sgf"], out=sgf, in_=bb, func=AF.Exp)
                ACTV(["bb"], ["lfb"], out=lfb, in_=bb, func=AF.Exp, scale=-1.0)
                V("dve", "tensor_tensor", ["qs", "sgf"], ["qt"], out=qt, in0=qs, in1=sgf, op=ALU.mult)
                V("dve", "tensor_tensor", ["kk", "lfb"], ["kt"], out=kt, in0=kk, in1=lfb, op=ALU.mult)
                V("dve", "tensor_tensor", ["kt", "sgf"], ["kh"], out=kh.rearrange("p (g s) -> p g s", s=32),
                  in0=kt.rearrange("p (g s) -> p g s", s=32),
                  in1=sgf.rearrange("p (g s) -> p g s", s=32)[:, :, 31:32].to_broadcast([128, 16, 32]), op=ALU.mult)
                stop_at("B2")
            if part == "pro":
                return
            hook(0)
            H4 = lambda ap: ap.rearrange("p (h t) -> p h t", h=4)
            STb, kst = pb.next()
            for h in range(4):
                MM(STb[:, h * 128:(h + 1) * 128], [(qkT[:, 4 + h, :], qkT[:, h, :])], ["qkT"], [kst])
            KTb, kkt = pt.next()
            for h in range(4):
                TR(KTb[:, h * 128:(h + 1) * 128], qkT[:, 4 + h, :], ["qkT"], [kkt], sig=(h == 3))
            ATb, kat = pb.next()
            for h in range(4):
                MM(ATb[:, h * 128:(h + 1) * 128], [(kt[:, h * 128:(h + 1) * 128], qt[:, h * 128:(h + 1) * 128])], ["kt", "qt"], [kat])
            KHb, kkh = pt.next()
            for h in range(4):
                TR(KHb[:, h * 128:(h + 1) * 128], kh[:, h * 128:(h + 1) * 128], ["kh"], [kkh], sig=(h == 3))
            for h in range(4):
                V("dve", "scalar_tensor_tensor", [kst, "aw", "tri"], [("STm", h)], out=STm[:, h, :], in0=STb[:, h * 128:(h + 1) * 128],
                  scalar=aw[:, h:h + 1], in1=tri[:], op0=ALU.mult, op1=ALU.mult)
            for h in range(4):
                ACTV([kkt, "aw"], [("kw", h)], out=kw[:, h, :], in_=KTb[:, h * 128:(h + 1) * 128], func=AF.Copy, scale=aw[:, 4 + h:5 + h])
            S.op("act", lambda e, KHb=KHb: e.copy(out=khT, in_=H4(KHb[:, 0:512])), [kkh], ["khT"])
            V("dve", "tensor_tensor", [kat, "blk"], ["ATm"], out=ATm, in0=H4(ATb[:, :]),
              in1=blk[:, :].unsqueeze(1).to_broadcast([128, 4, 128]), op=ALU.mult)
            Qbs = []
            for h in range(4):
                Qb, kqb = Qblk_r.next()
                V("dve", "tensor_tensor", ["qt", "mt"], [kqb], out=Qb,
                  in0=qt[:, h * 128:(h + 1) * 128].unsqueeze(1).to_broadcast([128, 4, 128]), in1=mt[:], op=ALU.mult)
                Qbs.append((Qb, kqb))
            V("dve", "tensor_copy", ["sgf"], ["decs"], out=decs, in_=sgf.rearrange("p (g s) -> p g s", s=32)[:, :, 31])
            hook(1)
            PPs = []
            for h in range(4):
                PP, kpp = pb.next()
                for c in range(4):
                    MM(PP[:, c * 128:(c + 1) * 128], [(khT[:, h, :], Vblk[:, c, h * 128:(h + 1) * 128])], ["khT", Kvb], [kpp])
                PPs.append((PP, kpp))
            for c in range(4):
                for h in range(4):
                    PP, kpp = PPs[h]
                    ksf, ksb = ("Sf", h), ("Sb", h)
                    V("dve", "scalar_tensor_tensor", [ksf, "decs", kpp], [ksf], out=Sf[:, h, :], in0=Sf[:, h, :],
                      scalar=decs[:, h * 4 + c:h * 4 + c + 1], in1=PP[:, c * 128:(c + 1) * 128], op0=ALU.mult, op1=ALU.add)
                    S.op("act", lambda e, h=h, c=c: e.copy(out=Sb[:, h, par * 4 + c, :], in_=Sf[:, h, :]), [ksf], [ksb])
            NUs = []
            for hp in range(2):
                NU, knu = pb.next()
                for hh in range(2):
                    h = 2 * hp + hh
                    MM(NU[:, hh * 129:(hh + 1) * 129], [(STm[:, h, :], vext[:, h, 0:129]), (qkT[:, h, :], Cb[:, h, 0:129])],
                       [("STm", h), Kv, "qkT", "Cb"], [knu])
                NUs.append((NU, knu))
            for hp in range(2):
                NU, knu = NUs[hp]
                V("dve", "tensor_tensor", [knu, "ebc"], ["dn"], out=dn[:, 2 * hp:2 * hp + 2],
                  in0=NU[:, 0:258].rearrange("p (h e) -> p h e", h=2)[:, :, 128], in1=ebc[:, 2 * hp:2 * hp + 2], op=ALU.mult)
            V("dve", "tensor_tensor", ["dn"], ["dn"], out=dn, in0=dn, in1=dn, op=ALU.mult)
            V("dve", "tensor_scalar", ["dn"], ["dn"], out=dn, in0=dn, scalar1=1.0, scalar2=None, op0=ALU.max)
            ACTV(["dn"], ["dn"], out=dn, in_=dn, func=AF.Ln)
            ACTV(["dn"], ["dn"], out=dn, in_=dn, func=AF.Exp, scale=-0.5)
            V("dve", "tensor_tensor", ["dn", "ebc"], ["sc"], out=sc, in0=dn, in1=ebc, op=ALU.mult)
            for h in range(4):
                NU, knu = NUs[h // 2]
                o_ = (h % 2) * 129
                V("dve", "scalar_tensor_tensor", [knu, "sc", Kso], ["hm"], out=hm[:, h, :], in0=NU[:, o_:o_ + 128],
                  scalar=sc[:, h:h + 1], in1=sig_o[:, h * 128:(h + 1) * 128], op0=ALU.mult, op1=ALU.mult)
                ACTV(["hm"], ["junk", "ssm"], out=junk[:, 0:128], in_=hm[:, h, :], func=AF.Square, accum_out=ssm[:, h:h + 1])
            hook(2)
            CUs = []
            for hp in range(2):
                CU, kcu = pb.next()
                for hh in range(2):
                    h = 2 * hp + hh
                    MM(CU[:, hh * 129:(hh + 1) * 129], [(kw[:, h, :], vext[:, h, 0:129])], [("kw", h), Kv], [kcu])
                CUs.append((CU, kcu))
            OO, koo = pb.next()
            for h in range(4):
                Qb, kqb = Qbs[h]
                pairs = [(ATm[:, h, :], hvb[:, h * 128:(h + 1) * 128]), (Qb[:, 0, :], Sb[:, h, (1 - par) * 4 + 3, :])]
                pairs += [(Qb[:, c, :], Sb[:, h, par * 4 + c - 1, :]) for c in range(1, 4)]
                MM(OO[:, h * 128:(h + 1) * 128], pairs, ["ATm", Khv, kqb, ("Sb", h)], [koo])
            for hp in range(2):
                CU, kcu = CUs[hp]
                for hh in range(2):
                    h = 2 * hp + hh
                    V("dve", "scalar_tensor_tensor", ["Cst", "ebdec", kcu], ["Cst"], out=Cst[:, h, :], in0=Cst[:, h, :],
                      scalar=ebdec[:, 4 + h:5 + h], in1=CU[:, hh * 129:(hh + 1) * 129], op0=ALU.mult, op1=ALU.add)
            S.op("act", lambda e: e.copy(out=Cb[:, :, 0:129], in_=Cst), ["Cst"], ["Cb"])
            for h in range(4):
                ACTV([koo], ["junk", "ssh"], out=junk[:, 0:128], in_=OO[:, h * 128:(h + 1) * 128], func=AF.Square,
                     accum_out=ssh[:, h:h + 1])
            V("dve", "tensor_tensor", [koo, Kgs], ["og"], out=og, in0=OO[:, :], in1=gs, op=ALU.mult)
            hook(3)
            rstd_from_ss(ssm, "ssm", 128, 4)
            rstd_from_ss(ssh, "ssh", 128, 4)
            for h in range(4):
                V("dve", "scalar_tensor_tensor", ["hm", "ssm", "gm_bc"], ["yb16"], out=yb16[:, h * 128:(h + 1) * 128],
                  in0=hm[:, h, :], scalar=ssm[:, h:h + 1], in1=gm_bc[:, h * 128:(h + 1) * 128], op0=ALU.mult, op1=ALU.mult)
            V("dve", "tensor_tensor", ["og", "ssh"], ["yb16"], out=H4(yb16[:, 512:1024]), in0=H4(og),
              in1=ssh.unsqueeze(2).to_broadcast([128, 4, 128]), op=ALU.mult)

        def stageC(i):
            xt, kx, hT, khT = state.pop(i)
            if dbg:
                yf = AFt[:, NF - 1024:NF]
                V("dve", "tensor_copy", ["yb16"], ["dbgy"], out=yf, in_=yb16)
                S.dma("sp", lambda e: e.dma_start(out=dbg_d["d_y"][i * 128:(i + 1) * 128, :], in_=yf), ["dbgy"], [], semkey=("dma", "dbgy"), final=True)
            transpose8(yb16, "yb16", yT, "yT", "act")
            for half in range(2):
                B, kb = pb.next()
                MM(B[:, 0:512], [(yT[:, k, :], Wo3[:, k, half * 512:(half + 1) * 512]) for k in range(8)], ["yT", "Wo"], [kb])
                V("dve", "tensor_tensor", [kb, kx], [kx], out=xt[:, half * 512:(half + 1) * 512], in0=B[:, 0:512],
                  in1=xt[:, half * 512:(half + 1) * 512], op=ALU.add)
            S.dma("sp", lambda e: e.dma_start(out=x1s[i * 128:(i + 1) * 128, :], in_=xt), [kx], [], semkey=("dma", "st", kx))
            if dbg:
                S.dma("sp", lambda e: e.dma_start(out=dbg_d["d_x1"][i * 128:(i + 1) * 128, :], in_=xt), [kx], [], semkey=("dma", "dbgx", kx), final=True)
            h2, kh2 = h2_r.next()
            ACTV([kx], [kh2, "ss2"], out=h2, in_=xt, func=AF.Square, accum_out=ss2)
            rstd_from_ss(ss2, "ss2", D, 1)
            V("dve", "scalar_tensor_tensor", [kx, "ss2", "gB"], [kh2], out=h2, in0=xt, scalar=ss2, in1=gB[:],
              op0=ALU.mult, op1=ALU.mult)
            transpose8(h2, kh2, h2T, "h2T", "act")
            S.call(lambda: S.pump(10 ** 9))
            ro = (i % 2) * 256
            RL = rbank[:, ro:ro + 64]; krl = ("pb", "r", i % 2)
            RK = rbank[:, ro + 64:ro + 128]; krk = krl
            MM(RL[:, 0:36], [(h2T[:, k, :], Wr[:, k, :]) for k in range(8)], ["h2T", "Wr"], [krl])
            _cap = S.capture()
            _lst = _cap.__enter__()
            V("dve", "tensor_tensor", [krl, "brt_bc"], ["lg"], out=lg, in0=RL[:, 0:36], in1=brt_bc[:], op=ALU.add)
            if dbg:
                S.dma("sp", lambda e: e.dma_start(out=dbg_d["d_lg"][i * 128:(i + 1) * 128, :], in_=lg), ["lg"], [], semkey=("dma", "dbglg"), final=True)
            V("dve", "tensor_reduce", ["lg"], ["gmax"], out=gmax, in_=lg[:, 0:4], axis=AX.X, op=ALU.max)
            V("dve", "tensor_scalar", ["gmax"], ["ngmax"], out=ngmax, in0=gmax, scalar1=-1.0, scalar2=None, op0=ALU.mult)
            ACTV(["lg", "ngmax"], ["ge", "gsum"], out=ge, in_=lg[:, 0:4], func=AF.Exp, bias=ngmax, scale=1.0, accum_out=gsum)
            V("dve", "reciprocal", ["gsum"], ["gval"], out=gval, in_=gsum)
            V("dve", "tensor_scalar", ["lg", "gmax"], ["G4"], out=G4, in0=lg[:, 0:4], scalar1=gmax, scalar2=None, op0=ALU.is_equal)
            V("dve", "tensor_scalar", ["G4"], ["pen"], out=pen, in0=G4, scalar1=1e30, scalar2=-1e30, op0=ALU.mult, op1=ALU.add)
            V("dve", "tensor_tensor", ["lg", "pen"], ["mskt"], out=mskt.rearrange("p (g j) -> p g j", g=4),
              in0=lg[:, 4:36].rearrange("p (g j) -> p g j", g=4), in1=pen.unsqueeze(2).to_broadcast([128, 4, 8]), op=ALU.add)
            V("dve", "max", ["mskt"], ["top8"], out=top8, in_=mskt)
            V("dve", "tensor_scalar", ["mskt", "top8"], ["sel1"], out=sel1, in0=mskt, scalar1=top8[:, 0:1], scalar2=None, op0=ALU.is_equal)
            V("dve", "tensor_scalar", ["mskt", "top8"], ["sel2"], out=sel2, in0=mskt, scalar1=top8[:, 1:2], scalar2=None, op0=ALU.is_equal)
            V("dve", "tensor_tensor", ["sel1", "sel2"], ["selb"], out=selb, in0=sel1, in1=sel2, op=ALU.add)
            V("dve", "tensor_tensor", ["top8"], ["dd"], out=dd, in0=top8[:, 1:2], in1=top8[:, 0:1], op=ALU.subtract)
            ACTV(["dd"], ["e2"], out=e2, in_=dd, func=AF.Exp)
            V("dve", "tensor_scalar", ["e2"], ["w1"], out=w1, in0=e2, scalar1=1.0, scalar2=None, op0=ALU.add)
            V("dve", "reciprocal", ["w1"], ["w1"], out=w1, in_=w1)
            V("dve", "tensor_tensor", ["w1", "gval"], ["cw1"], out=cw1[:, i:i + 1], in0=w1, in1=gval, op=ALU.mult)
            V("dve", "tensor_tensor", ["cw1", "e2"], ["cw2"], out=cw2[:, i:i + 1], in0=cw1[:, i:i + 1], in1=e2, op=ALU.mult)
            MM(RK[:, 0:32], [(stri[:], selb)], ["stri", "selb"], [krk])
            MM(RK[:, 32:64], [(onesb[:], selb)], ["onesb", "selb"], [krk])
            V("dve", "tensor_tensor", [krk, "base"], ["pos"], out=pos, in0=RK[:, 0:32], in1=base[:], op=ALU.add)
            V("dve", "tensor_tensor", [krk, "base"], ["base"], out=base[:], in0=RK[:, 32:64], in1=base[:], op=ALU.add)
            V("dve", "tensor_scalar", ["pos"], ["tmp32"], out=tmp32, in0=pos, scalar1=float(CAP), scalar2=1e9, op0=ALU.is_ge, op1=ALU.mult)
            V("dve", "tensor_tensor", ["pos", "ec"], ["pos"], out=pos, in0=pos, in1=ec[:], op=ALU.add)
            V("dve", "tensor_tensor", ["pos", "tmp32"], ["pos"], out=pos, in0=pos, in1=tmp32, op=ALU.add)
            V("dve", "tensor_scalar", ["pos"], ["pos"], out=pos, in0=pos, scalar1=float(ZROW), scalar2=None, op0=ALU.min)
            V("dve", "tensor_tensor", ["pos", "sel1"], ["sel1"], out=sel1, in0=sel1, in1=pos, op=ALU.mult)
            V("dve", "tensor_tensor", ["pos", "sel2"], ["sel2"], out=sel2, in0=sel2, in1=pos, op=ALU.mult)
            V("dve", "tensor_reduce", ["sel1"], ["d1f"], out=d1f, in_=sel1, axis=AX.X, op=ALU.add)
            V("dve", "tensor_reduce", ["sel2"], ["d2f"], out=d2f, in_=sel2, axis=AX.X, op=ALU.add)
            V("dve", "tensor_copy", ["d1f"], [("d1i", i)], out=d1i[:, i:i + 1], in_=d1f)
            V("dve", "tensor_copy", ["d2f"], [("d2i", i)], out=d2i[:, i:i + 1], in_=d2f)
            if dbg:
                sl = AFt[:, NF - 1028:NF - 1024]
                V("dve", "tensor_copy", ["d1f"], ["dbgs"], out=sl[:, 0:1], in_=d1f)
                V("dve", "tensor_copy", ["d2f"], ["dbgs"], out=sl[:, 1:2], in_=d2f)
                V("dve", "tensor_copy", ["cw1"], ["dbgs"], out=sl[:, 2:3], in_=cw1[:, i:i + 1])
                V("dve", "tensor_copy", ["cw2"], ["dbgs"], out=sl[:, 3:4], in_=cw2[:, i:i + 1])
                S.dma("sp", lambda e: e.dma_start(out=dbg_d["d_slot"][i * 128:(i + 1) * 128, :], in_=sl), ["dbgs"], [], semkey=("dma", "dbgs"), final=True)
            for di, kd in ((d1i, ("d1i", i)), (d2i, ("d2i", i))):
                S.dma("pool", lambda e, di=di, h2=h2: e.indirect_dma_start(
                    out=xs_d, out_offset=bass.IndirectOffsetOnAxis(ap=di[:, i:i + 1], axis=0), in_=h2, in_offset=None),
                    [kh2, kd], [], semkey=("dma", "scat", kh2, kd[0]))
            _cap.__exit__(None, None, None)
            S.call(lambda _lst=_lst: S.deferred.extend(_lst))

        try:
            A_pre(0)
            for grp in AORDER:
                for nm in grp:
                    A_grp(0, nm)
            conv_silu(0)
            stageB(0, part="pro")

            def hook_for(i):
                def hk(n):
                    for nm in AORDER[n]:
                        A_grp(i + 1, nm)
                    if n == 2:
                        conv_silu(i + 1)
                return hk

            def zip2(la, lb):
                na, nb = len(la), len(lb)
                ia = ib = 0
                while ia < na or ib < nb:
                    if ib >= nb or (ia < na and ia * nb <= ib * na):
                        la[ia](); ia += 1
                    else:
                        lb[ib](); ib += 1
            for i in range(NT):
                if i + 1 < NT:
                    A_pre(i + 1)
                    hk = hook_for(i)
                else:
                    hk = lambda n: None
                stageB(i, hk, part="main")
                with S.capture() as lc_:
                    stageC(i)
                lp_ = []
                if i + 1 < NT:
                    with S.capture() as lp_:
                        stageB(i + 1, part="pro")
                zip2(lc_, lp_)
        except StopBuild:
            pass
        if STOP in ("A", "B", "C", "P1"):
            S.pump(10 ** 9)
            S.flush()
            return nc

        S.barrier()
        AFa.reset(); ABa.reset()
        NWE = 8 * 512
        wslots = []
        for s in range(2):
            o = s * 3 * NWE
            wslots.append((Wi[:, o:o + NWE].rearrange("p (k n) -> p k n", k=8),
                           Wi[:, o + NWE:o + 2 * NWE].rearrange("p (k n) -> p k n", k=8),
                           Wi[:, o + 2 * NWE:o + 3 * NWE].rearrange("p (f n) -> p f n", f=4)))
        Wpg = Wi[:, 6 * NWE:6 * NWE + 8 * D].rearrange("p (k n) -> p k n", k=8)
        Wpp = Wo[:, 0:2 * D].rearrange("p (k n) -> p k n", k=2)
        xr_r = ABa.ring("xr", [128, NB, D], 2); xT_r = ABa.ring("xT", [128, 8, CAP], 2); act_r = ABa.ring("actT", [128, 4, CAP], 2)
        yo_r = AFa.ring("yo", [128, D], 2); sgt_r = AFa.ring("sgt", [128, CAP], 2); stg_r = AFa.ring("stg", [128, 4096], 2)

        wsrc = lambda e: (("g", weg[e].rearrange("(p k) n -> p k n", k=8), 8),
                          ("u", weu[e].rearrange("(p k) n -> p k n", k=8), 8),
                          ("d", wed[e].rearrange("(p f) n -> p f n", f=4), 4))
        staged = {}

        def stage_load(e, j):
            nm, src, kk_ = wsrc(e)[j]
            st, kst = stg_r.next()
            st3 = st.rearrange("p (k n) -> p k n", k=kk_)
            LD("sp", st3, src, [kst])
            staged[(e, j)] = (st3, kst, kk_, nm)

        def cast_w(e, j):
            st3, kst, kk_, nm = staged.pop((e, j))
            dst = wslots[e % 2][j]
            kq = ("wexp", e % 2)
            h_ = kk_ // 2
            S.op("act", lambda en, dst=dst, st3=st3, h_=h_: en.copy(out=dst[:, 0:h_, :], in_=st3[:, 0:h_, :]), [kst], [kq + (nm, 0)])
            V("dve", "tensor_copy", [kst], [kq + (nm, 1)], out=dst[:, h_:, :], in_=st3[:, h_:, :])

        def load_expert(e):
            stage_load(e, 0); stage_load(e, 1); cast_w(e, 0); stage_load(e, 2); cast_w(e, 1); cast_w(e, 2)

        load_expert(0)
        stage_load(1, 0); stage_load(1, 1)
        LD("pool", Wpg, wplg.rearrange("(k p) n -> p k n", p=128), ["Wpg"])
        LD("pool", Wpp, wplp.rearrange("(k p) n -> p k n", p=128), ["Wpp"])
        LD("sp", gA[:], g_pl.partition_broadcast(128), ["gA"])
        LD("sp", gB[:], g_fin.partition_broadcast(128), ["gB"])
        def prep_rows(e):
            xr, kxr = xr_r.next()
            LD("pool", xr, xs_d[e * CAP:(e + 1) * CAP, :].rearrange("(b p) d -> p b d", p=128), [kxr])
            xT, kxT = xT_r.next()
            for b in range(NB):
                P, kp = pt.next()
                for k in range(8):
                    TR(P[:, k * 128:(k + 1) * 128], xr[:, b, k:D:8], [kxr], [kp], sig=(k == 7))
                if b % 2 == 0:
                    S.op("act", lambda en, P=P, b=b, xT=xT: en.copy(out=xT[:, :, b * 128:(b + 1) * 128],
                                                                   in_=P[:, :].rearrange("p (k t) -> p k t", k=8)), [kp], [kxT])
                else:
                    V("dve", "tensor_copy", [kp], [kxT], out=xT[:, :, b * 128:(b + 1) * 128],
                      in_=P[:, :].rearrange("p (k t) -> p k t", k=8))
            return xT, kxT

        xt_cur = prep_rows(0)
        for e in range(NE):
            wg, wu, wd = wslots[e % 2]
            kq = ("wexp", e % 2)
            xT, kxT = xt_cur
            if e + 1 < NE:
                cast_w(e + 1, 0); cast_w(e + 1, 1); stage_load(e + 1, 2)
            aT, kaT = act_r.next()
            for f in range(4):
                Gp, kg = pb.next()
                MM(Gp[:, 0:CAP], [(wg[:, k, f:DE:4], xT[:, k, :]) for k in range(8)], [kq + ("g", 0), kq + ("g", 1), kxT], [kg])
                Up, ku = pb.next()
                MM(Up[:, 0:CAP], [(wu[:, k, f:DE:4], xT[:, k, :]) for k in range(8)], [kq + ("u", 0), kq + ("u", 1), kxT], [ku])
                sg, ksg = sgt_r.next()
                ACTV([kg], [ksg], out=sg, in_=Gp[:, 0:CAP], func=AF.Silu)
                V("dve", "tensor_tensor", [ksg, ku], [kaT], out=aT[:, f, :], in0=sg, in1=Up[:, 0:CAP], op=ALU.mult)
            if e + 1 < NE:
                cast_w(e + 1, 2)
            if e + 2 < NE:
                stage_load(e + 2, 0); stage_load(e + 2, 1)
            if e + 1 < NE:
                xt_cur = prep_rows(e + 1)
            for b in range(NB):
                yo, kyo = yo_r.next()
                for half in range(2):
                    Yp, ky = pb.next()
                    MM(Yp[:, 0:512], [(aT[:, f, b * 128:(b + 1) * 128], wd[:, f, half * 512:(half + 1) * 512]) for f in range(4)],
                       [kaT, kq + ("d", 0), kq + ("d", 1)], [ky])
                    if half == 0:
                        S.op("act", lambda en, yo=yo, Yp=Yp: en.copy(out=yo[:, 0:512], in_=Yp[:, 0:512]), [ky], [kyo])
                    else:
                        V("dve", "tensor_copy", [ky], [kyo], out=yo[:, 512:1024], in_=Yp[:, 0:512])
                r0 = e * CAP + b * 128
                S.dma("act", lambda en, yo=yo, r0=r0: en.dma_start(out=yb_d[r0:r0 + 128, :], in_=yo), [kyo], [], semkey=("dma", "st", kyo))

        if STOP == "P2":
            S.flush()
            return nc
        S.barrier()
        AFa.reset(); ABa.reset()
        x1_r = AFa.ring("x1", [128, D], 3); y1_r = AFa.ring("y1", [128, D], 2); y2_r = AFa.ring("y2", [128, D], 2)
        pf_r = AFa.ring("pf", [128, 256], 3); sg3_r = AFa.ring("sg3", [128, D], 1); ot_r = AFa.ring("ot", [128, D], 2)
        sm3_r = AFa.ring("sm3", [128, 8], 3)
        h3_r = ABa.ring("h3", [128, D], 2); h3T_r = ABa.ring("h3T", [128, 8, 128], 2); pbf_r = ABa.ring("pbf", [128, 256], 2)
        pT_r = ABa.ring("pT", [128, 2, 128], 2); junk3 = ABa.get([128, D])
        p3_ld = {}

        def p3_loads(i):
            x1, kx1 = x1_r.next(); pf, kpf = pf_r.next()
            LD("sp", x1, x1s[i * 128:(i + 1) * 128, :], [kx1])
            LD("sp", pf, p_d[i * 128:(i + 1) * 128, :], [kpf])
            p3_ld[i] = (x1, kx1, pf, kpf)

        def p3_front(i):
            y1, ky1 = y1_r.next(); y2, ky2 = y2_r.next()
            sg3, ksg3 = sg3_r.next(); sm3, ksm3 = sm3_r.next(); h3, kh3 = h3_r.next(); pbf, kpbf = pbf_r.next()
            ss3 = sm3[:, 0:1]; ss4 = sm3[:, 1:2]; kss3 = ksm3 + ("a",); kss4 = ksm3 + ("b",)
            if i == 0:
                p3_loads(0)
            if i + 1 < NT:
                p3_loads(i + 1)
            x1, kx1, pf, kpf = p3_ld.pop(i)
            for yy, kyy, di in ((y1, ky1, d1i), (y2, ky2, d2i)):
                S.dma("pool", lambda e, yy=yy, di=di, i=i: e.indirect_dma_start(
                    out=yy, out_offset=None, in_=yb_d, in_offset=bass.IndirectOffsetOnAxis(ap=di[:, i:i + 1], axis=0)),
                    ["yb_z"], [kyy])
            V("dve", "scalar_tensor_tensor", [kx1, ky1], [kx1], out=x1, in0=y1, scalar=cw1[:, i:i + 1], in1=x1, op0=ALU.mult, op1=ALU.add)
            V("dve", "scalar_tensor_tensor", [kx1, ky2], [kx1], out=x1, in0=y2, scalar=cw2[:, i:i + 1], in1=x1, op0=ALU.mult, op1=ALU.add)
            ACTV([kx1], ["junk3", kss3], out=junk3, in_=x1, func=AF.Square, accum_out=ss3)
            rstd_from_ss(ss3, kss3, D, 1)
            V("dve", "scalar_tensor_tensor", [kx1, kss3, "gA"], [kh3], out=h3, in0=x1, scalar=ss3, in1=gA[:], op0=ALU.mult, op1=ALU.mult)
            h3T, kh3T = h3T_r.next()
            transpose8(h3, kh3, h3T, kh3T, "act")
            S.op("act", lambda e, pf=pf, pbf=pbf: e.copy(out=pbf, in_=pf), [kpf], [kpbf])
            P, kp = pt.next()
            for k in range(2):
                TR(P[:, k * 128:(k + 1) * 128], pbf[:, k * 128:(k + 1) * 128], [kpbf], [kp], sig=(k == 1))
            pT, kpT = pT_r.next()
            V("dve", "tensor_copy", [kp], [kpT], out=pT, in_=P[:, 0:256].rearrange("p (k t) -> p k t", k=2))
            return dict(x1=x1, kx1=kx1, sg3=sg3, ksg3=ksg3, sm3=sm3, ksm3=ksm3, h3T=h3T, kh3T=kh3T, pT=pT, kpT=kpT)

        def p3_back(i, c_):
            x1, kx1, sg3, ksg3, sm3, ksm3 = c_['x1'], c_['kx1'], c_['sg3'], c_['ksg3'], c_['sm3'], c_['ksm3']
            h3T, kh3T, pT, kpT = c_['h3T'], c_['kh3T'], c_['pT'], c_['kpT']
            ss4 = sm3[:, 1:2]; kss4 = ksm3 + ('b',)
            for half in range(2):
                Gp, kg = pb.next()
                MM(Gp[:, 0:512], [(h3T[:, k, :], Wpg[:, k, half * 512:(half + 1) * 512]) for k in range(8)], [kh3T, "Wpg"], [kg])
                Pp, kpp = pb.next()
                MM(Pp[:, 0:512], [(pT[:, k, :], Wpp[:, k, half * 512:(half + 1) * 512]) for k in range(2)], [kpT, "Wpp"], [kpp])
                ACTV([kg], [ksg3], out=sg3[:, half * 512:(half + 1) * 512], in_=Gp[:, 0:512], func=AF.Sigmoid)
                V("dve", "tensor_tensor", [ksg3, kpp], [ksg3], out=sg3[:, half * 512:(half + 1) * 512],
                  in0=sg3[:, half * 512:(half + 1) * 512], in1=Pp[:, 0:512], op=ALU.mult)
            V("dve", "tensor_tensor", [ksg3, kx1], [kx1], out=x1, in0=x1, in1=sg3, op=ALU.add)
            ACTV([kx1], ["junk3", kss4], out=junk3, in_=x1, func=AF.Square, accum_out=ss4)
            rstd_from_ss(ss4, kss4, D, 1)
            ot, kot = ot_r.next()
            V("dve", "scalar_tensor_tensor", [kx1, kss4, "gB"], [kot], out=ot, in0=x1, scalar=ss4, in1=gB[:], op0=ALU.mult, op1=ALU.mult)
            S.dma("sp", lambda e, ot=ot, i=i: e.dma_start(out=out_d[i * 128:(i + 1) * 128, :], in_=ot), [kot], [], semkey=("dma", "st", kot), final=True)

        def zip_emit(la, lb):
            na, nb = len(la), len(lb)
            ia = ib = 0
            while ia < na or ib < nb:
                if ib >= nb or (ia < na and ia * nb <= ib * na):
                    la[ia](); ia += 1
                else:
                    lb[ib](); ib += 1

        ctx3 = p3_front(0)
        for i in range(NT):
            nxt = None
            lf_ = []
            if i + 1 < NT:
                with S.capture() as lf_:
                    nxt = p3_front(i + 1)
            with S.capture() as lb_:
                p3_back(i, ctx3)
            zip_emit(lb_, lf_)
            ctx3 = nxt
        S.flush()
    return nc


def _consts(CAP):
    s = np.arange(128)
    tri = (s[:, None] <= s[None, :]).astype(np.float32)
    stri = (s[:, None] < s[None, :]).astype(np.float32)
    blk = tri * ((s[:, None] // 32) == (s[None, :] // 32))
    rmask = np.tile(((np.arange(512) % 32) != 0).astype(np.float32)[None, :], (128, 1))
    mt = np.zeros((128, 4, 128), np.float32)
    ms = np.zeros((128, 4), np.float32)
    for c in range(4):
        mt[:, c, c * 32:(c + 1) * 32] = 1.0
        ms[c * 32:(c + 1) * 32, c] = 1.0
    ec = np.tile((np.arange(32) * CAP).astype(np.float32)[None, :], (128, 1))
    return {"c_ident": np.eye(128, dtype=np.float32), "c_tri": tri, "c_blk": blk.astype(np.float32), "c_stri": stri,
            "c_rmask": rmask, "c_mt": mt, "c_ms": ms, "c_ec": ec}


def make_in_maps(inp, n_cores, seqs_per_core):
    f = lambda a: np.ascontiguousarray(np.asarray(a, dtype=np.float32))
    x = f(inp["x"]); p = f(inp["p"])[0]
    B, T, _ = x.shape
    shared = {
        "w_in": f(inp["w_in"])[0], "w_out": f(inp["w_out"])[0],
        "wrt": f(np.concatenate([inp["w_rg"][0], inp["w_re"][0]], axis=1)),
        "brt": f(np.concatenate([inp["b_rg"][0], inp["b_re"][0]], axis=0))[None, :],
        "cw": f(np.asarray(inp["conv_qk"])[0].T.reshape(8, 128, 4).transpose(1, 0, 2)),
        "hl": f(np.asarray(inp["hg_lb"]).reshape(2, 4, 128).transpose(2, 1, 0)),
        "g_mix": f(inp["g_mix"])[0:1], "g_ffn": f(inp["g_ffn"])[0:1], "g_pl": f(inp["g_pl"])[0:1],
        "g_final": f(inp["g_final"])[None, :], "g_mlstm": f(inp["g_mlstm"])[0:1], "g_hgrn": f(inp["g_hgrn"])[0:1],
        "b_mgate": f(inp["b_mgate"])[0:1],
        "w_e_gate": f(inp["w_e_gate"])[0], "w_e_up": f(inp["w_e_up"])[0], "w_e_down": f(inp["w_e_down"])[0],
        "w_pl_gate": f(inp["w_pl_gate"])[0], "w_pl_proj": f(inp["w_pl_proj"])[0],
    }
    maps = []
    for c in range(n_cores):
        b0 = c * seqs_per_core
        m = dict(shared)
        m["x"] = f(x[b0:b0 + seqs_per_core].reshape(seqs_per_core * T, D))
        m["p"] = f(p[b0:b0 + seqs_per_core].reshape(seqs_per_core * T, 256))
        maps.append(m)
    return maps


def run(inp, n_cores, seqs_per_core, CAP, dbg=False):
    x = np.asarray(inp["x"])
    B, T, _ = x.shape
    TPS = T // 128
    NT = TPS * seqs_per_core
    nc = build_nc(NT, TPS, CAP, dbg=dbg)
    maps = make_in_maps(inp, n_cores, seqs_per_core)
    cst = _consts(CAP)
    for m in maps:
        m.update(cst)
    res = run_bass_kernel_spmd(nc, maps, core_ids=list(range(n_cores)))
    out = np.concatenate([np.asarray(r["out"]).reshape(seqs_per_core, T, D) for r in res.results], axis=0)
    if dbg:
        return out.astype(np.float32), res.results
    return out.astype(np.float32)


def kernel(**inputs):
    return run(inputs, 8, 2, 512)
```
